# Optimizing a Trainium2 kernel written in Bass

```python
import math
import jax, jax.numpy as jnp
from jax import lax
import numpy as np

D_MODEL = 1024
BATCH = 1
SEQ = 16384
DEPTH = 4

HEAD_DIM = 64
A_HEADS = 8
MOBA_BLOCK = 256
MOBA_TOPK = 3
MOBA_QCHUNK = 64
B_HEADS = 8
B_KV_HEADS = 2
B_WINDOW = 128
C_GROUPS = ((128, 1), (512, 4), (2048, 16))
N_C_GROUPS = 3
C_HEADS_PER_GROUP = 4
C_HEADS = N_C_GROUPS * C_HEADS_PER_GROUP
C_MAX_DILATION = 16
BAND = 128
D_FF = ((8 * D_MODEL + 3 * 256 - 1) // (3 * 256)) * 256
A_QKV = 3 * A_HEADS * HEAD_DIM
B_Q = B_HEADS * HEAD_DIM
B_KV = 2 * B_KV_HEADS * HEAD_DIM
C_QKV = 3 * C_HEADS * HEAD_DIM
N_BRANCH = 3
GATE_W = N_BRANCH * D_MODEL
IN_WIDTH = A_QKV + B_Q + B_KV + C_QKV + GATE_W
N_ALIBI_HEADS = A_HEADS + B_HEADS + C_HEADS
SEQ_MULTIPLE = math.lcm(BAND * C_MAX_DILATION, MOBA_BLOCK, MOBA_QCHUNK)
RMS_EPS = 1e-6

kernel_name = "hybrid_moba_swa_dilated_gated_trunk"


def alibi_slopes():
    n = N_ALIBI_HEADS
    s = 2.0 ** (-8.0 * (np.arange(n) + 1) / n)
    return jnp.asarray(s, dtype=jnp.float32)


def rmsnorm(x, g):
    xf = x.astype(jnp.float32)
    y = xf * lax.rsqrt(jnp.mean(xf * xf, axis=-1, keepdims=True) + RMS_EPS)
    return (y * g.astype(jnp.float32)).astype(x.dtype)


def banded_attention(q, k, v, slopes, max_steps, dilation, sinks=None):
    Bn, S, Hq, Dh = q.shape
    Hkv = k.shape[2]
    G = Hq // Hkv
    L = S // dilation
    nb = L // BAND

    def split(a):
        h = a.shape[2]
        a = a.reshape(Bn, L, dilation, h, Dh).transpose(0, 2, 1, 3, 4)
        return a.reshape(Bn, dilation, nb, BAND, h, Dh)

    def with_prev(a):
        prev = jnp.concatenate([jnp.zeros_like(a[:, :, :1]), a[:, :, :-1]], axis=2)
        return jnp.concatenate([prev, a], axis=3)

    qb = split(q).reshape(Bn, dilation, nb, BAND, Hkv, G, Dh)
    kk = with_prev(split(k))
    vv = with_prev(split(v))
    s = jnp.einsum('brnihgd,brnjhd->brnhgij', qb, kk).astype(jnp.float32) * (Dh ** -0.5)
    i = jnp.arange(BAND)[:, None]
    j = jnp.arange(2 * BAND)[None, :]
    steps = i + BAND - j
    blk = jnp.arange(nb)[:, None, None]
    mask = (steps >= 0) & (steps <= max_steps) & ((blk > 0) | (j >= BAND))
    bias = -slopes.reshape(Hkv, G)[:, :, None, None] * (steps * dilation).astype(jnp.float32)
    s = jnp.where(mask[:, None, None], s + bias, -jnp.inf)
    m = jnp.max(s, axis=-1)
    if sinks is not None:
        sink = sinks.reshape(Hkv, G, 1).astype(jnp.float32)
        m = jnp.maximum(m, sink)
    p = jnp.exp(s - m[..., None])
    denom = jnp.sum(p, axis=-1)
    if sinks is not None:
        denom = denom + jnp.exp(sink - m)
    o = jnp.einsum('brnhgij,brnjhd->brnihgd', p.astype(v.dtype), vv).astype(jnp.float32)
    denom_t = denom.transpose(0, 1, 2, 5, 3, 4)
    o = o / denom_t[..., None]
    lse = m.transpose(0, 1, 2, 5, 3, 4) + jnp.log(denom_t)
    o = o.reshape(Bn, dilation, L, Hq, Dh).transpose(0, 2, 1, 3, 4).reshape(Bn, S, Hq, Dh)
    lse = lse.reshape(Bn, dilation, L, Hq).transpose(0, 2, 1, 3).reshape(Bn, S, Hq)
    return o, lse


def moba_attention(q, k, v, slopes):
    Bn, S, H, Dh = q.shape
    nblk = S // MOBA_BLOCK
    ksel = min(MOBA_TOPK, nblk)
    nc = S // MOBA_QCHUNK
    scale = Dh ** -0.5
    kbh = k.transpose(0, 2, 1, 3).reshape(Bn, H, nblk, MOBA_BLOCK, Dh)
    vbh = v.transpose(0, 2, 1, 3).reshape(Bn, H, nblk, MOBA_BLOCK, Dh)
    kmean = jnp.mean(kbh.astype(jnp.float32), axis=3)
    gate = jnp.einsum('bshd,bhnd->bhsn', q.astype(jnp.float32), kmean)
    own = jnp.arange(S) // MOBA_BLOCK
    past = jnp.arange(nblk)[None, :] < own[:, None]
    gate = jnp.where(past, gate, -jnp.inf)
    _, sel = lax.top_k(gate, ksel)
    q_c = q.reshape(Bn, nc, MOBA_QCHUNK, H, Dh).transpose(1, 0, 3, 2, 4)
    sel_c = sel.reshape(Bn, H, nc, MOBA_QCHUNK, ksel).transpose(2, 0, 1, 3, 4)
    bi = jnp.arange(Bn)[:, None, None, None]
    hi = jnp.arange(H)[None, :, None, None]
    slope = slopes.astype(jnp.float32)[None, :, None, None]

    def chunk(args):
        ci, qc, sc = args
        t = ci * MOBA_QCHUNK + jnp.arange(MOBA_QCHUNK)
        own_blk = (ci * MOBA_QCHUNK) // MOBA_BLOCK
        k_sel = kbh[bi, hi, sc]
        v_sel = vbh[bi, hi, sc]
        k_own = lax.dynamic_index_in_dim(kbh, own_blk, axis=2, keepdims=False)
        v_own = lax.dynamic_index_in_dim(vbh, own_blk, axis=2, keepdims=False)
        pos_sel = sc[..., None] * MOBA_BLOCK + jnp.arange(MOBA_BLOCK)
        valid = (jnp.arange(ksel)[None, :] < (t // MOBA_BLOCK)[:, None])[None, None, :, :, None]
        s_sel = jnp.einsum('bhqd,bhqjkd->bhqjk', qc, k_sel).astype(jnp.float32) * scale
        s_sel = jnp.where(valid, s_sel - slope[..., None] * (t[:, None, None] - pos_sel).astype(jnp.float32), -jnp.inf)
        pos_own = own_blk * MOBA_BLOCK + jnp.arange(MOBA_BLOCK)
        d_own = t[:, None] - pos_own[None, :]
        s_own = jnp.einsum('bhqd,bhkd->bhqk', qc, k_own).astype(jnp.float32) * scale
        s_own = jnp.where(d_own >= 0, s_own - slope * d_own.astype(jnp.float32), -jnp.inf)
        s_all = jnp.concatenate([s_sel.reshape(Bn, H, MOBA_QCHUNK, ksel * MOBA_BLOCK), s_own], axis=-1)
        p = jax.nn.softmax(s_all, axis=-1).astype(v.dtype)
        p_sel = p[..., :ksel * MOBA_BLOCK].reshape(Bn, H, MOBA_QCHUNK, ksel, MOBA_BLOCK)
        p_own = p[..., ksel * MOBA_BLOCK:]
        return (jnp.einsum('bhqjk,bhqjkd->bhqd', p_sel, v_sel)
                + jnp.einsum('bhqk,bhkd->bhqd', p_own, v_own))

    o = lax.map(chunk, (jnp.arange(nc), q_c, sel_c))
    return o.transpose(1, 0, 3, 2, 4).reshape(Bn, S, H, Dh)


def dilated_attention(q, k, v, slopes):
    outs, lses = [], []
    for g, (w, d) in enumerate(C_GROUPS):
        o, lse = banded_attention(q[:, :, g], k[:, :, g], v[:, :, g],
                                  slopes[g * C_HEADS_PER_GROUP:(g + 1) * C_HEADS_PER_GROUP], w // d, d)
        outs.append(o)
        lses.append(lse)
    wts = jax.nn.softmax(jnp.stack(lses, axis=0), axis=0)
    return jnp.sum(wts[..., None] * jnp.stack(outs, axis=0), axis=0)


def token_mixers(u, w_in, sinks, w_br_a, w_br_b, w_br_c, w_out, slopes):
    Bn, S, _ = u.shape
    z = jnp.dot(u, w_in)
    o1 = A_QKV
    o2 = o1 + B_Q
    o3 = o2 + B_KV
    o4 = o3 + C_QKV
    za, zbq, zbkv, zc, zg = jnp.split(z, [o1, o2, o3, o4], axis=-1)
    slopes_b = slopes[:B_HEADS]
    slopes_c = slopes[B_HEADS:B_HEADS + C_HEADS]
    slopes_a = slopes[B_HEADS + C_HEADS:]
    za = za.reshape(Bn, S, 3, A_HEADS, HEAD_DIM)
    oa = moba_attention(za[:, :, 0], za[:, :, 1], za[:, :, 2], slopes_a).astype(u.dtype)
    ya = jnp.dot(oa.reshape(Bn, S, A_HEADS * HEAD_DIM), w_br_a)
    qb = zbq.reshape(Bn, S, B_HEADS, HEAD_DIM)
    zbkv = zbkv.reshape(Bn, S, 2, B_KV_HEADS, HEAD_DIM)
    ob, _ = banded_attention(qb, zbkv[:, :, 0], zbkv[:, :, 1], slopes_b, B_WINDOW - 1, 1, sinks)
    yb = jnp.dot(ob.astype(u.dtype).reshape(Bn, S, B_HEADS * HEAD_DIM), w_br_b)
    zc = zc.reshape(Bn, S, N_C_GROUPS, 3, C_HEADS_PER_GROUP, HEAD_DIM)
    oc = dilated_attention(zc[:, :, :, 0], zc[:, :, :, 1], zc[:, :, :, 2], slopes_c).astype(u.dtype)
    yc = jnp.dot(oc.reshape(Bn, S, C_HEADS_PER_GROUP * HEAD_DIM), w_br_c)
    gates = jax.nn.sigmoid(zg.astype(jnp.float32)).astype(u.dtype).reshape(Bn, S, N_BRANCH, D_MODEL)
    merged = gates[:, :, 0] * ya + gates[:, :, 1] * yb + gates[:, :, 2] * yc
    return jnp.dot(merged, w_out)


def swiglu(u, w_gate, w_up, w_down):
    return jnp.dot(jax.nn.silu(jnp.dot(u, w_gate)) * jnp.dot(u, w_up), w_down)


def setup_inputs(seed: int = 0) -> dict:
    key = jax.random.key(seed)
    ks = jax.random.split(key, 20)
    f32 = jnp.float32
    D = D_MODEL

    def nrm(k, shape, scale):
        return jax.random.normal(k, shape, f32) * scale

    return {
        "x": nrm(ks[0], (BATCH, SEQ, D), 1.0),
        "c": nrm(ks[1], (BATCH, D), 1.0),
        "w_ada": nrm(ks[2], (DEPTH, D, 6 * D), 0.5 * D ** -0.5),
        "b_ada": nrm(ks[3], (DEPTH, 6 * D), 0.01),
        "g_pre_mix": 1.0 + nrm(ks[4], (DEPTH, D), 0.05),
        "g_post_mix": 1.0 + nrm(ks[5], (DEPTH, D), 0.05),
        "w_in": nrm(ks[6], (DEPTH, D, IN_WIDTH), D ** -0.5),
        "sinks": nrm(ks[7], (DEPTH, B_HEADS), 1.0),
        "w_br_a": nrm(ks[8], (DEPTH, A_HEADS * HEAD_DIM, D), (A_HEADS * HEAD_DIM) ** -0.5),
        "w_br_b": nrm(ks[9], (DEPTH, B_HEADS * HEAD_DIM, D), (B_HEADS * HEAD_DIM) ** -0.5),
        "w_br_c": nrm(ks[10], (DEPTH, C_HEADS_PER_GROUP * HEAD_DIM, D), (C_HEADS_PER_GROUP * HEAD_DIM) ** -0.5),
        "w_out": nrm(ks[11], (DEPTH, D, D), D ** -0.5),
        "g_pre_ffn": 1.0 + nrm(ks[12], (DEPTH, D), 0.05),
        "g_post_ffn": 1.0 + nrm(ks[13], (DEPTH, D), 0.05),
        "w_gate": nrm(ks[14], (DEPTH, D, D_FF), D ** -0.5),
        "w_up": nrm(ks[15], (DEPTH, D, D_FF), D ** -0.5),
        "w_down": nrm(ks[16], (DEPTH, D_FF, D), D_FF ** -0.5),
    }


def reference(x, c, w_ada, b_ada, g_pre_mix, g_post_mix, w_in, sinks, w_br_a, w_br_b, w_br_c,
              w_out, g_pre_ffn, g_post_ffn, w_gate, w_up, w_down):
    Bn, S, _ = x.shape
    S_pad = -(-S // SEQ_MULTIPLE) * SEQ_MULTIPLE
    h = jnp.pad(x, ((0, 0), (0, S_pad - S), (0, 0)))
    slopes = alibi_slopes()
    for l in range(DEPTH):
        mod = jnp.dot(jax.nn.silu(c), w_ada[l]) + b_ada[l]
        sh1, sc1, gt1, sh2, sc2, gt2 = [m[:, None, :] for m in jnp.split(mod, 6, axis=-1)]
        u = rmsnorm(h, g_pre_mix[l]) * (1.0 + sc1) + sh1
        y = token_mixers(u, w_in[l], sinks[l], w_br_a[l], w_br_b[l], w_br_c[l], w_out[l], slopes)
        h = h + gt1 * rmsnorm(y, g_post_mix[l])
        u = rmsnorm(h, g_pre_ffn[l]) * (1.0 + sc2) + sh2
        y = swiglu(u, w_gate[l], w_up[l], w_down[l])
        h = h + gt2 * rmsnorm(y, g_post_ffn[l])
    return h[:, :S]
```

```python
from contextlib import ExitStack
import os
import numpy as np
import ml_dtypes
import concourse.bass as bass
import concourse.mybir as mybir
from concourse.bass_utils import run_bass_kernel_spmd

F32 = mybir.dt.float32
BF16 = mybir.dt.bfloat16
AF = mybir.ActivationFunctionType
ALU = mybir.AluOpType
AX = mybir.AxisListType

COMPUTE = ("pe", "act", "dve", "pool")
NDMASEM = 8


class Res:
    __slots__ = ("name", "w", "r", "excl")

    def __init__(self, name, excl=False):
        self.name = name
        self.excl = excl
        self.w = None
        self.r = []


class Sched:
    def __init__(self, nc, stack, dma_queues=("sp", "pool")):
        self.nc = nc
        self.ops = {e: [] for e in ("pe", "act", "dve", "pool", "sp")}
        self.sem = {}
        self.cnt = {}
        for e in COMPUTE:
            self.sem[e] = stack.enter_context(nc.semaphore("s_" + e))
            self.cnt[e] = 0
        self.dsem = {}
        self.dcnt = {}
        self.dnext = {}
        for q in dma_queues:
            self.dsem[q] = [stack.enter_context(nc.semaphore("d_%s%d" % (q, i))) for i in range(NDMASEM)]
            self.dcnt[q] = [0] * NDMASEM
            self.dnext[q] = 0
        self.waited = {}
        self.out_tokens = []

    def _waits(self, eng, deps):
        need = {}
        for d in deps:
            if d is None:
                continue
            semkey, val, prod = d
            if prod == "pe" and eng == "pe":
                continue
            if need.get(semkey, 0) < val:
                need[semkey] = val
        waits = []
        for semkey, val in need.items():
            key = (eng, semkey)
            if self.waited.get(key, 0) < val:
                self.waited[key] = val
                waits.append((semkey, val))
        return waits

    def _semof(self, semkey):
        if semkey[0] == "c":
            return self.sem[semkey[1]]
        return self.dsem[semkey[1]][semkey[2]]

    def _deps(self, reads, writes, extra):
        deps = list(extra)
        for r in reads:
            deps.append(r.w)
            if r.excl:
                deps.extend(r.r)
        for w in writes:
            deps.append(w.w)
            deps.extend(w.r)
        return deps

    def _commit(self, tok, reads, writes):
        for r in reads:
            if r.excl:
                r.r = [tok]
            else:
                r.r.append(tok)
        for w in writes:
            w.w = tok
            w.r = []

    def op(self, eng, fn, reads=(), writes=(), extra=()):
        waits = self._waits(eng, self._deps(reads, writes, extra))
        self.cnt[eng] += 1
        tok = (("c", eng), self.cnt[eng], eng)
        self.ops[eng].append((waits, fn, (("c", eng), 1)))
        self._commit(tok, reads, writes)
        return tok

    def dma(self, q, fn, reads=(), writes=(), extra=()):
        i = self.dnext[q]
        self.dnext[q] = (i + 1) % NDMASEM
        semkey = ("d", q, i)
        deps = self._deps(reads, writes, extra)
        if self.dcnt[q][i] > 0:
            deps.append((semkey, self.dcnt[q][i], "dma"))
        waits = self._waits(q, deps)
        self.dcnt[q][i] += 16
        tok = (semkey, self.dcnt[q][i], "dma")
        self.ops[q].append((waits, fn, (semkey, 16)))
        self._commit(tok, reads, writes)
        return tok

    def emit(self, final_tokens):
        nc = self.nc
        fw = self._waits("sp", final_tokens)
        with nc.Block() as block:
            def run(engname):
                def body(eng):
                    for waits, fn, inc in self.ops[engname]:
                        for semkey, val in waits:
                            eng.wait_ge(self._semof(semkey), val)
                        fn(eng).then_inc(self._semof(inc[0]), inc[1])
                    if engname == "sp":
                        for semkey, val in fw:
                            eng.wait_ge(self._semof(semkey), val)
                return body
            block.sync(run("sp"))
            block.tensor(run("pe"))
            block.scalar(run("act"))
            block.vector(run("dve"))
            block.gpsimd(run("pool"))


S_TOK = 16384
NCH = 32
BIG = 30000.0
NEG = -30000.0


def alibi_slopes_np():
    n = 28
    return (2.0 ** (-8.0 * (np.arange(n) + 1) / n)).astype(np.float64)


def att_consts(c):
    sl = alibi_slopes_np()
    slope_a = sl[20 + c]
    ik = np.arange(128)[:, None]
    didx = np.arange(131)[None, :]
    biasA = (slope_a * (ik - 128.0 * (didx - 3))).astype(np.float32)
    cm = np.zeros((128, 4, 512), np.float32)
    for e in range(4):
        kp = 128 * e + np.arange(128)[:, None]
        qp = np.arange(512)[None, :]
        kb = e // 2
        qb = qp // 256
        cm[:, e, :] = np.where(qb == kb, (kp <= qp), (kb < qb)).astype(np.float32)
    hc = c % 4
    units = [(sl[c], 1, 127), (sl[8 + hc], 1, 128), (sl[12 + hc], 4, 128), (sl[16 + hc], 16, 128)]
    bu = np.zeros((128, 4, 2, 128), np.float32)
    j = np.arange(128)[:, None]
    i = np.arange(128)[None, :]
    for u, (slope, d, ms) in enumerate(units):
        for part in range(2):
            steps = i + 128 - j if part == 0 else i - j
            ok = (steps >= 0) & (steps <= ms)
            bu[:, u, part, :] = np.where(ok, -slope * steps * d, NEG)
    onehot = np.zeros((64, S_TOK), np.float32)
    onehot[np.arange(S_TOK) // 256, np.arange(S_TOK)] = 1.0
    return dict(biasA=biasA, cmask=cm.reshape(128, 2048).astype(ml_dtypes.bfloat16),
                biasU=bu.reshape(128, 1024), onehot=onehot.astype(ml_dtypes.bfloat16),
                ident=np.eye(128, dtype=np.float32))


def att_weights(w_in_l, sinks_l, c):
    hc = c % 4
    def A(part, h): return w_in_l[:, part * 512 + h * 64: part * 512 + h * 64 + 64]
    def Bq(h): return w_in_l[:, 1536 + h * 64: 1536 + h * 64 + 64]
    def Bkv(part, j): return w_in_l[:, 2048 + part * 128 + j * 64: 2048 + part * 128 + j * 64 + 64]
    def C(g, part, h): return w_in_l[:, 2304 + g * 768 + part * 256 + h * 64: 2304 + g * 768 + part * 256 + h * 64 + 64]
    kvh = c // 4
    wfm = np.concatenate([A(1, c), A(0, c),
                          Bq(c), C(0, 0, hc),
                          Bkv(0, kvh), C(0, 1, hc),
                          C(1, 0, hc), C(2, 0, hc),
                          C(1, 1, hc), C(2, 1, hc)], axis=1)
    wv = np.concatenate([A(2, c), Bkv(1, kvh), C(0, 2, hc), C(1, 2, hc), C(2, 2, hc)], axis=1)
    sink = np.full((128, 1), sinks_l[c], np.float32)
    return dict(wfm=np.ascontiguousarray(wfm), wv=np.ascontiguousarray(wv), sink=sink)


def build_att(n_chunks=NCH):
    nc = bass.Bass("TRN2", target_bir_lowering=False)
    D = lambda name, shape, dt, kind="ExternalInput": nc.dram_tensor(name, shape, dt, kind=kind).ap()
    uT = D("uT", [1024, S_TOK], BF16)
    wfm = D("wfm", [1024, 640], F32)
    wv = D("wv", [1024, 320], F32)
    onehot = D("onehot", [64, S_TOK], BF16)
    biasA_d = D("biasA", [128, 131], F32)
    cmask_d = D("cmask", [128, 2048], BF16)
    biasU_d = D("biasU", [128, 1024], F32)
    sink_d = D("sink", [128, 1], F32)
    ident_d = D("ident", [128, 128], F32)
    oa_d = D("oa", [64, S_TOK], BF16, "ExternalOutput")
    ob_d = D("ob", [64, S_TOK], BF16, "ExternalOutput")
    oc_d = D("oc", [64, S_TOK], BF16, "ExternalOutput")

    with ExitStack() as st:
        S = Sched(nc, st)
        sb = lambda name, shape, dt: st.enter_context(nc.sbuf_tensor(name, shape, dt))
        pst = lambda name: st.enter_context(nc.psum_tensor(name, [128, 512], F32))
        KT = sb("KT", [128, S_TOK], BF16)
        VA = sb("VA", [128, 128, 128], BF16)
        usb = sb("usb", [128, 8, 2048], BF16)
        wfm_sb = sb("wfm_sb", [128, 8, 640], BF16)
        wv_sb = sb("wv_sb", [128, 8, 320], BF16)
        Q1 = sb("Q1", [128, 2048], BF16)
        Q23 = sb("Q23", [128, 2048], BF16)
        K1 = [sb("K1_%d" % i, [128, 2048], BF16) for i in range(2)]
        K23 = [sb("K23_%d" % i, [128, 2048], BF16) for i in range(2)]
        VU = [[sb("VU%d_%d" % (u, i), [128, 16, 128], BF16) for i in range(2)] for u in range(4)]
        accC = sb("accC", [128, 2048], F32)
        scb = sb("scb", [128, 2, 512], F32)
        Pb = sb("Pb", [128, 2, 512], BF16)
        PT = [sb("PT%d" % i, [128, 512], BF16) for i in range(2)]
        QTA = [sb("QTA%d" % i, [128, 512], BF16) for i in range(2)]
        QTF = [sb("QTF%d" % i, [64, 512], F32) for i in range(2)]
        ksum = sb("ksum", [64, 64], F32)
        gs = sb("gs", [128, 4, 64], F32)
        top8 = sb("top8", [128, 4, 8], F32)
        Mtok = sb("Mtok", [128, 4, 128], F32)
        biasA = sb("biasA_sb", [128, 131], F32)
        cmask = sb("cmask_sb", [128, 4, 512], BF16)
        biasU = sb("biasU_sb", [128, 4, 2, 128], F32)
        sink = sb("sink_sb", [128, 1], F32)
        esink = sb("esink", [128, 1], F32)
        ident = sb("ident_sb", [128, 128], F32)
        rden = sb("rden", [128, 512], F32)
        oa_sb = [sb("oa_sb%d" % i, [64, 512], BF16) for i in range(2)]
        ob_sb = [sb("ob_sb%d" % i, [64, 512], BF16) for i in range(2)]
        oc_sb = ob_sb
        psP = [pst("psP0"), pst("psP1")]
        psS = [pst("psS0"), pst("psS1")]
        psO = pst("psO")
        psB = [pst("psB0"), pst("psB1")]
        psBO = pst("psBO")

        R = Res
        r_w = R("w"); r_const = R("const"); r_ones = R("ones")
        r_u = [R("u%d" % i) for i in range(4)]
        r_kt = [R("kt%d" % i) for i in range(NCH)]
        r_va = [R("va%d" % i) for i in range(NCH)]
        r_ks = [R("ks%d" % i) for i in range(NCH)]
        r_q1 = [R("q1") for i in range(4)]; r_q23 = [R("q23") for i in range(4)]
        r_k1 = [[R("k1") for i in range(4)] for s in range(2)]
        r_k23 = [[R("k23") for i in range(4)] for s in range(2)]
        r_vu = [[[R("vu") for i in range(4)] for s in range(2)] for u in range(4)]
        r_acc = R("acc"); r_sc = R("sc"); r_Pb = R("Pb")
        r_PT = [R("PT0"), R("PT1")]
        r_qta = [R("qta0"), R("qta1")]; r_qtf = [R("qtf0"), R("qtf1")]
        r_gs = R("gs"); r_top8 = R("top8"); r_M = R("M")
        r_rden = R("rden"); r_oa = [R("oa0"), R("oa1")]; r_ob = [R("ob0"), R("ob1")]
        r_oc = r_ob
        r_psP = [R("psP0", True), R("psP1", True)]; r_psS = [R("psS0", True), R("psS1", True)]; r_psO = R("psO", True)
        r_psB = [R("psB0", True), R("psB1", True)]; r_psBO = R("psBO", True)
        r_esink = R("esink")

        import os
        DBG = int(os.environ.get("ATT_DBG", "99"))
        S.dma("pool", lambda e: e.dma_start(out=wfm_sb[:], in_=wfm.rearrange("(kc p) m -> p kc m", p=128)), writes=[r_w])
        S.dma("pool", lambda e: e.dma_start(out=wv_sb[:], in_=wv.rearrange("(kc p) m -> p kc m", p=128)), writes=[r_w])
        S.dma("sp", lambda e: e.dma_start(out=KT[64:128, :], in_=onehot), writes=[r_const])
        S.dma("sp", lambda e: e.dma_start(out=biasA[:], in_=biasA_d), writes=[r_const])
        S.dma("sp", lambda e: e.dma_start(out=cmask[:], in_=cmask_d.rearrange("p (e q) -> p e q", e=4)), writes=[r_const])
        S.dma("sp", lambda e: e.dma_start(out=biasU[:], in_=biasU_d.rearrange("p (u a q) -> p u a q", u=4, a=2)), writes=[r_const])
        S.dma("sp", lambda e: e.dma_start(out=sink[:], in_=sink_d), writes=[r_const])
        S.dma("sp", lambda e: e.dma_start(out=ident[:], in_=ident_d), writes=[r_const])
        if DBG >= 2:
            S.op("pool", lambda e: e.memset(VA[:, :, 64:128], 1.0), writes=[r_ones])
        for u in range(4 if DBG >= 2 else 0):
            for s in range(2):
                S.op("pool", lambda e, u=u, s=s: e.memset(VU[u][s][:, :, 64:128], 1.0), writes=[r_ones])
        S.op("pool", lambda e: e.memset(ksum[:], 0.0), writes=[r_ks[0]])
        S.op("pool", lambda e: e.memset(Mtok[:], 0.0), writes=[r_M])
        S.op("act", lambda e: e.activation(out=esink[:], in_=sink[:], func=AF.Exp), reads=[r_const], writes=[r_esink])

        out_toks = []
        evq = [0]

        def evac(out, in_, reads, writes):
            evq[0] += 1
            if evq[0] % 2 == 0:
                return S.op("dve", lambda e: e.tensor_copy(out=out, in_=in_), reads=reads, writes=writes)
            return S.op("act", lambda e: e.activation(out=out, in_=in_, func=AF.Copy), reads=reads, writes=writes)

        def dve_copy(out, in_, reads, writes):
            return S.op("dve", lambda e: e.tensor_copy(out=out, in_=in_), reads=reads, writes=writes)

        pcount = [0]

        def next_psP():
            pcount[0] += 1
            return pcount[0] % 2

        def proj(t):
            ci = t % 4
            s = t // 4
            ss = s % 2
            off = 512 * ci
            col = 512 * t
            q = "sp" if t % 2 == 0 else "pool"
            S.dma("sp", lambda e: e.dma_start(out=usb[:, :, off:off + 512],
                                              in_=uT.rearrange("(kc p) n -> p kc n", p=128)[:, :, col:col + 512]),
                  writes=[r_u[ci]])
            slot = t % 2
            for mc in range(5 if DBG >= 4 else 0):
                if mc == 0 and DBG < 5:
                    continue
                x = next_psP()
                for kc in range(8):
                    S.op("pe", lambda e, x=x, kc=kc, mc=mc: e.matmul(psP[x][:], wfm_sb[:, kc, mc * 128:(mc + 1) * 128],
                                                                     usb[:, kc, off:off + 512], start=(kc == 0), stop=(kc == 7)),
                         reads=[r_w, r_u[ci]], writes=[r_psP[x]])
                if mc == 0:
                    S.op("act", lambda e, x=x: e.activation(out=KT[0:64, col:col + 512], in_=psP[x][0:64, :], func=AF.Copy),
                         reads=[r_psP[x]], writes=[r_kt[t]])
                    dve_copy(QTA[slot][0:64, :], psP[x][64:128, :], [r_psP[x]], [r_qta[slot]])
                    dve_copy(QTF[slot][0:64, :], psP[x][64:128, :], [r_psP[x]], [r_qtf[slot]])
                    S.op("dve", lambda e, x=x: e.tensor_reduce(out=ksum[:, 2 * t:2 * t + 2],
                                                               in_=psP[x][0:64, :].rearrange("p (a b) -> p a b", b=256),
                                                               axis=AX.X, op=ALU.add),
                         reads=[r_psP[x]], writes=[r_ks[t]])
                elif mc == 1:
                    evac(Q1[:, off:off + 512], psP[x][:], [r_psP[x]], [r_q1[ci]])
                elif mc == 2:
                    evac(K1[ss][:, off:off + 512], psP[x][:], [r_psP[x]], [r_k1[ss][ci]])
                elif mc == 3:
                    evac(Q23[:, off:off + 512], psP[x][:], [r_psP[x]], [r_q23[ci]])
                else:
                    evac(K23[ss][:, off:off + 512], psP[x][:], [r_psP[x]], [r_k23[ss][ci]])
            for p in range(2 if DBG >= 6 else 0):
                x = next_psP()
                for j in range(2):
                    tl = 2 * p + j
                    for kc in range(8):
                        S.op("pe", lambda e, x=x, kc=kc, j=j, tl=tl: e.matmul(
                            psP[x][:, j * 192:(j + 1) * 192], usb[:, kc, off + 128 * tl: off + 128 * tl + 128],
                            wv_sb[:, kc, 0:192], start=(kc == 0), stop=(kc == 7)),
                            reads=[r_w, r_u[ci]], writes=[r_psP[x]])
                pv = psP[x][:, 0:384].rearrange("p (j c) -> p j c", j=2)
                ta = 4 * t + 2 * p
                tb = 4 * ci + 2 * p
                evac(VA[:, ta:ta + 2, 0:64], pv[:, :, 0:64], [r_psP[x]], [r_va[t]])
                evac(VU[0][ss][:, tb:tb + 2, 0:64], pv[:, :, 64:128], [r_psP[x]], [r_vu[0][ss][ci]])
                evac(VU[1][ss][:, tb:tb + 2, 0:64], pv[:, :, 128:192], [r_psP[x]], [r_vu[1][ss][ci]])

        def maskprep(t):
            slot = t % 2
            x = next_psP()
            nb = 2 * t + 1
            for qt in range(4):
                S.op("pe", lambda e, x=x, qt=qt: e.matmul(psP[x][:, qt * 64:(qt + 1) * 64], QTF[slot][0:64, qt * 128:(qt + 1) * 128],
                                                          ksum[:, :], start=True, stop=True),
                     reads=[r_qtf[slot]] + r_ks[0:t + 1], writes=[r_psP[x]])
            S.op("dve", lambda e, x=x: e.tensor_copy(out=gs[:], in_=psP[x][:, 0:256].rearrange("p (a b) -> p a b", b=64)),
                 reads=[r_psP[x]], writes=[r_gs])
            for half in range(2):
                b = 2 * t + half
                qs = slice(2 * half, 2 * half + 2)
                if b >= 4:
                    if b < 8:
                        S.op("dve", lambda e, b=b, qs=qs: e.memset(gs[:, qs, b:8], -1e30), reads=[], writes=[r_gs])
                    w = max(b, 8)
                    for qt in range(2 * half, 2 * half + 2):
                        S.op("dve", lambda e, qt=qt, w=w: e.max(out=top8[:, qt, :], in_=gs[:, qt, 0:w]), reads=[r_gs], writes=[r_top8])
                        S.op("dve", lambda e, qt=qt, b=b: e.tensor_scalar(out=Mtok[:, qt, 64:64 + b], in0=gs[:, qt, 0:b],
                                                                          scalar1=top8[:, qt, 2:3], scalar2=BIG,
                                                                          op0=ALU.is_ge, op1=ALU.mult),
                             reads=[r_gs, r_top8], writes=[r_M])
                elif b > 0:
                    S.op("dve", lambda e, qs=qs, b=b: e.memset(Mtok[:, qs, 64:64 + b], BIG), writes=[r_M])
                S.op("dve", lambda e, qs=qs, b=b: e.memset(Mtok[:, qs, 64 + b:64 + b + 1], BIG), writes=[r_M])
                if b + 1 < 64:
                    S.op("dve", lambda e, qs=qs, b=b: e.memset(Mtok[:, qs, 64 + b + 1:128], 0.0), writes=[r_M])
            y = next_psP()
            for qt in range(4):
                S.op("pe", lambda e, y=y, qt=qt: e.matmul(psP[y][:, qt * 128:(qt + 1) * 128], Mtok[:, qt, :], ident[:], start=True, stop=True),
                     reads=[r_M, r_const], writes=[r_psP[y]])
            S.op("dve", lambda e, y=y: e.tensor_scalar_add(out=QTA[slot][64:128, :], in0=psP[y][64:128, :], scalar1=-BIG),
                 reads=[r_psP[y]], writes=[r_qta[slot]])

        def moba(t):
            slot = t % 2
            nkt = 4 * t + 4
            def pv(kt):
                p = kt % 2
                S.op("pe", lambda e: e.matmul(psO[:], VA[:, kt, :], PT[p][:], start=(kt == 0), stop=(kt == nkt - 1)),
                     reads=[r_va[kt // 4], r_PT[p], r_ones], writes=[r_psO])
            for kt in range(nkt):
                p = kt % 2
                S.op("pe", lambda e, kt=kt, p=p: e.matmul(psS[p][:], KT[:, kt * 128:(kt + 1) * 128], QTA[slot][:], start=True, stop=True),
                     reads=[r_kt[kt // 4], r_const, r_qta[slot]], writes=[r_psS[p]])
                di = 4 * t - kt + 3
                S.op("act", lambda e, p=p, di=di: e.activation(out=PT[p][:], in_=psS[p][:], func=AF.Exp, bias=biasA[:, di:di + 1], scale=0.125),
                     reads=[r_psS[p], r_const], writes=[r_PT[p]])
                if kt >= 4 * t:
                    ee = kt - 4 * t
                    S.op("pool", lambda e, p=p, ee=ee: e.tensor_tensor(out=PT[p][:], in0=PT[p][:], in1=cmask[:, ee, :], op=ALU.mult),
                         reads=[r_const, r_PT[p]], writes=[r_PT[p]])
                if kt >= 1:
                    pv(kt - 1)
            pv(nkt - 1)
            S.op("dve", lambda e: e.reciprocal(out=rden[64:128, :], in_=psO[64:128, :]), reads=[r_psO], writes=[r_rden])
            S.op("dve", lambda e: e.tensor_tensor(out=oa_sb[slot][:], in0=psO[0:64, :], in1=rden[64:128, :], op=ALU.mult),
                 reads=[r_psO, r_rden], writes=[r_oa[slot]])
            out_toks.append(S.dma("sp", lambda e: e.dma_start(out=oa_d[:, 512 * t:512 * t + 512], in_=oa_sb[slot][:]), reads=[r_oa[slot]]))

        def tile_cols(d, idx):
            if d == 1:
                return slice(128 * idx, 128 * idx + 128)
            if d == 4:
                r, m = idx // 4, idx % 4
                return slice(512 * m + r, 512 * m + 512, 4)
            return slice(idx, 2048, 16)

        def cis(d, idx):
            if d == 1:
                return [idx // 4]
            if d == 4:
                return [idx % 4]
            return [0, 1, 2, 3]

        def strided_v(s):
            ss = s % 2
            for u, d, wc in ((2, 4, 192), (3, 16, 256)):
                for half in range(2):
                    x = next_psP()
                    for j in range(8):
                        idx = half * 8 + j
                        cs = tile_cols(d, idx)
                        for kc in range(8):
                            S.op("pe", lambda e, x=x, j=j, kc=kc, cs=cs, wc=wc: e.matmul(
                                psP[x][:, j * 64:(j + 1) * 64], usb[:, kc, cs], wv_sb[:, kc, wc:wc + 64],
                                start=(kc == 0), stop=(kc == 7)), reads=[r_w] + r_u, writes=[r_psP[x]])
                    evac(VU[u][ss][:, half * 8:half * 8 + 8, 0:64], psP[x][:].rearrange("p (j c) -> p j c", c=64),
                         [r_psP[x]], r_vu[u][ss])

        UNITS = [(0, 1, Q1, K1, 0, r_q1, r_k1), (1, 1, Q1, K1, 64, r_q1, r_k1),
                 (2, 4, Q23, K23, 0, r_q23, r_k23), (3, 16, Q23, K23, 64, r_q23, r_k23)]

        def do_quad(s, unit, a):
            (u, d, Qb, Kb, rb, rq, rk) = unit
            ss = s % 2
            ps_ = 1 - ss
            rows = slice(rb, rb + 64)
            quad = [4 * a + j for j in range(4)]
            prevs = []
            for idx in quad:
                if d == 1:
                    pr = (ss, idx - 1) if idx > 0 else ((ps_, 15) if s > 0 else None)
                elif d == 4:
                    pr = (ss, idx - 1) if idx % 4 > 0 else ((ps_, idx + 3) if s > 0 else None)
                else:
                    pr = (ps_, idx) if s > 0 else None
                prevs.append(pr)
            anyprev = any(p is not None for p in prevs)
            for j, idx in enumerate(quad):
                cs = tile_cols(d, idx)
                rq_l = [rq[i] for i in cis(d, idx)]
                if prevs[j] is not None:
                    psl, pidx = prevs[j]
                    pcs = tile_cols(d, pidx)
                    S.op("pe", lambda e, j=j, psl=psl, pcs=pcs, cs=cs: e.matmul(psB[0][:, 128 * j:128 * j + 128], Kb[psl][rows, pcs], Qb[rows, cs], start=True, stop=True),
                         reads=rq_l + [rk[psl][i] for i in cis(d, pidx)], writes=[r_psB[0]])
                S.op("pe", lambda e, j=j, cs=cs: e.matmul(psB[1][:, 128 * j:128 * j + 128], Kb[ss][rows, cs], Qb[rows, cs], start=True, stop=True),
                     reads=rq_l + [rk[ss][i] for i in cis(d, idx)], writes=[r_psB[1]])
            j0 = min([j for j in range(4) if prevs[j] is not None], default=4)
            S.op("dve", lambda e: e.scalar_tensor_tensor(
                out=scb[:, 1, :].rearrange("p (j q) -> p j q", j=4),
                in0=psB[1][:].rearrange("p (j q) -> p j q", j=4), scalar=0.125,
                in1=biasU[:, u, 1:2, :].to_broadcast([128, 4, 128]),
                op0=ALU.mult, op1=ALU.add), reads=[r_psB[1], r_const], writes=[r_sc])
            if j0 < 4:
                S.op("dve", lambda e: e.scalar_tensor_tensor(
                    out=scb[:, 0, 128 * j0:512].rearrange("p (j q) -> p j q", q=128),
                    in0=psB[0][:, 128 * j0:512].rearrange("p (j q) -> p j q", q=128), scalar=0.125,
                    in1=biasU[:, u, 0:1, :].to_broadcast([128, 4 - j0, 128]),
                    op0=ALU.mult, op1=ALU.add), reads=[r_psB[0], r_const], writes=[r_sc])
            if j0 == 0:
                S.op("act", lambda e: e.activation(out=Pb[:], in_=scb[:], func=AF.Exp), reads=[r_sc], writes=[r_Pb])
            else:
                S.op("act", lambda e: e.activation(out=Pb[:, 1, :], in_=scb[:, 1, :], func=AF.Exp), reads=[r_sc], writes=[r_Pb])
                if j0 < 4:
                    S.op("act", lambda e: e.activation(out=Pb[:, 0, 128 * j0:512], in_=scb[:, 0, 128 * j0:512], func=AF.Exp), reads=[r_sc], writes=[r_Pb])
            for j, idx in enumerate(quad):
                has_prev = prevs[j] is not None
                if has_prev:
                    psl, pidx = prevs[j]
                    S.op("pe", lambda e, j=j, psl=psl, pidx=pidx: e.matmul(psBO[:, 128 * j:128 * j + 128], VU[u][psl][:, pidx, :], Pb[:, 0, 128 * j:128 * j + 128], start=True, stop=False),
                         reads=[r_Pb, r_ones] + r_vu[u][psl], writes=[r_psBO])
                S.op("pe", lambda e, j=j, idx=idx, has_prev=has_prev: e.matmul(psBO[:, 128 * j:128 * j + 128], VU[u][ss][:, idx, :], Pb[:, 1, 128 * j:128 * j + 128], start=(not has_prev), stop=True),
                     reads=[r_Pb, r_ones] + r_vu[u][ss], writes=[r_psBO])
            if u == 0:
                bs = (4 * s + a) % 2
                S.op("dve", lambda e: e.tensor_scalar_add(out=rden[64:128, :], in0=psBO[64:128, :], scalar1=esink[64:128, 0:1]),
                     reads=[r_psBO, r_esink], writes=[r_rden])
                S.op("dve", lambda e: e.reciprocal(out=rden[64:128, :], in_=rden[64:128, :]), reads=[r_rden], writes=[r_rden])
                S.op("dve", lambda e: e.tensor_tensor(out=ob_sb[bs][:], in0=psBO[0:64, :], in1=rden[64:128, :], op=ALU.mult),
                     reads=[r_psBO, r_rden], writes=[r_ob[bs]])
                c0 = 2048 * s + 512 * a
                out_toks.append(S.dma("sp", lambda e: e.dma_start(out=ob_d[:, c0:c0 + 512], in_=ob_sb[bs][:]), reads=[r_ob[bs]]))
            elif u == 1:
                dve_copy(accC[:, 512 * a:512 * a + 512], psBO[:], [r_psBO], [r_acc])
            elif u == 2:
                av = accC[:].rearrange("p (m i r) -> p m i r", m=4, i=128, r=4)[:, :, :, a]
                S.op("dve", lambda e: e.tensor_tensor(out=av, in0=av, in1=psBO[:].rearrange("p (m i) -> p m i", m=4), op=ALU.add),
                     reads=[r_psBO, r_acc], writes=[r_acc])
            else:
                av = accC[:].rearrange("p (i r) -> p i r", r=16)[:, :, 4 * a:4 * a + 4]
                S.op("dve", lambda e: e.tensor_tensor(out=av, in0=av, in1=psBO[:].rearrange("p (j i) -> p i j", j=4), op=ALU.add),
                     reads=[r_psBO, r_acc], writes=[r_acc])

        def finish_c(s):
            for a in range(4):
                bs = a % 2
                S.op("dve", lambda e, a=a: e.reciprocal(out=rden[0:64, :], in_=accC[64:128, 512 * a:512 * a + 512]), reads=[r_acc], writes=[r_rden])
                S.op("dve", lambda e, a=a, bs=bs: e.tensor_tensor(out=oc_sb[bs][:], in0=accC[0:64, 512 * a:512 * a + 512], in1=rden[0:64, :], op=ALU.mult),
                     reads=[r_acc, r_rden], writes=[r_oc[bs]])
                c0 = 2048 * s + 512 * a
                out_toks.append(S.dma("sp", lambda e, bs=bs, c0=c0: e.dma_start(out=oc_d[:, c0:c0 + 512], in_=oc_sb[bs][:]), reads=[r_oc[bs]]))

        def banded(s):
            for unit in UNITS:
                for a in range(4):
                    do_quad(s, unit, a)
            finish_c(s)

        import os
        parts = os.environ.get("ATT_PARTS", "mask,moba,band").split(",")
        for t in range(n_chunks):
            if DBG >= 3:
                proj(t)
            if "mask" in parts:
                maskprep(t)
            if "moba" in parts:
                moba(t)
            if t % 4 == 3 and "band" in parts:
                strided_v(t // 4)
                banded(t // 4)
        if not out_toks:
            out_toks.append(S.dma("sp", lambda e: e.dma_start(out=oa_d[:, 0:512], in_=QTA[0][0:64, :]), reads=[r_qta[0]]))
        S.emit(out_toks)
    return nc


TP = 1024
NPASS = 2
EPS = 1e-6
D_FF = 2816
NV = 13


def blockW(w, kc_rows=None):
    K, N = w.shape
    return np.ascontiguousarray(w.reshape(K // 128, 128, N // 128, 128).transpose(2, 1, 0, 3))


def colvec(v):
    return np.ascontiguousarray(v.reshape(-1, 128).T)


def build_mod():
    nc = bass.Bass("TRN2", target_bir_lowering=False)
    D = lambda name, shape, dt, kind="ExternalInput": nc.dram_tensor(name, shape, dt, kind=kind).ap()
    c_d = D("c_col", [128, 8], F32)
    w_d = D("wada", [6, 128, 8, 512], F32)
    b_d = D("bada", [128, 24], F32)
    o_d = D("modc", [128, 24], F32, "ExternalOutput")
    with ExitStack() as st:
        S = Sched(nc, st)
        sb = lambda name, shape, dt: st.enter_context(nc.sbuf_tensor(name, shape, dt))
        cc = sb("cc", [128, 8], F32)
        scc = sb("scc", [128, 8], F32)
        bb = sb("bb", [128, 24], F32)
        oo = sb("oo", [128, 24], F32)
        wb = [sb("wb%d" % i, [128, 8, 512], F32) for i in range(2)]
        ps = st.enter_context(nc.psum_tensor("ps", [128, 512], F32))
        r_c = Res("c"); r_sc = Res("sc"); r_b = Res("b"); r_o = Res("o"); r_ps = Res("ps", True)
        r_w = [Res("w0"), Res("w1")]
        S.dma("sp", lambda e: e.dma_start(out=cc[:], in_=c_d), writes=[r_c])
        S.dma("sp", lambda e: e.dma_start(out=bb[:], in_=b_d), writes=[r_b])
        S.op("act", lambda e: e.activation(out=scc[:], in_=cc[:], func=AF.Silu), reads=[r_c], writes=[r_sc])
        for blk in range(6):
            s = blk % 2
            S.dma("sp" if blk % 2 == 0 else "pool", lambda e, blk=blk, s=s: e.dma_start(out=wb[s][:], in_=w_d[blk]), writes=[r_w[s]])
            for j in range(4):
                n = blk * 4 + j
                for kc in range(8):
                    S.op("pe", lambda e, s=s, j=j, kc=kc, n=n: e.matmul(ps[:, n:n + 1], wb[s][:, kc, j * 128:(j + 1) * 128], scc[:, kc:kc + 1],
                                                                        start=(kc == 0), stop=(kc == 7)),
                         reads=[r_w[s], r_sc], writes=[r_ps])
        S.op("dve", lambda e: e.tensor_tensor(out=oo[:], in0=ps[:, 0:24], in1=bb[:], op=ALU.add), reads=[r_ps, r_b], writes=[r_o])
        t = S.dma("sp", lambda e: e.dma_start(out=o_d, in_=oo[:]), reads=[r_o])
        S.emit([t])
    return nc


def build_dense(do_main=True, do_next=True):
    nc = bass.Bass("TRN2", target_bir_lowering=False)
    D = lambda name, shape, dt, kind="ExternalInput": nc.dram_tensor(name, shape, dt, kind=kind).ap()
    h_d = D("hT", [NPASS, 128, 8, TP], F32)
    vec_d = D("vecs", [128, NV, 8], F32)
    ho_d = D("hT_out", [NPASS, 128, 8, TP], F32, "ExternalOutput")
    if do_next:
        uo_d = D("uT_out", [NPASS, 128, 8, TP], BF16, "ExternalOutput")
    if do_main:
        o_d = D("oT", [NPASS, 128, 10, TP], BF16)
        wg_d = D("w_g", [24, 128, 8, 128], F32)
        wbr_d = D("w_br", [8, 128, 10, 128], F32)
        wout_d = D("w_out", [8, 128, 8, 128], F32)
        wgate_d = D("w_gate", [22, 128, 8, 128], F32)
        wup_d = D("w_up", [22, 128, 8, 128], F32)
        wdown_d = D("w_down", [8, 128, 22, 128], F32)

    with ExitStack() as st:
        S = Sched(nc, st)
        sb = lambda name, shape, dt: st.enter_context(nc.sbuf_tensor(name, shape, dt))
        hT = sb("hT_sb", [128, 8, TP], F32)
        uT = sb("uT_sb", [128, 8, TP], BF16)
        vec = sb("vec", [128, NV, 8], F32)
        coef = sb("coef", [128, 8, 8], F32)
        ones = sb("ones", [128, 128], BF16)
        sq = sb("sq", [128, 8, 512], BF16)
        rstd = sb("rstd", [128, 512], F32)
        tmp = [sb("tmp%d" % i, [128, 512], F32) for i in range(2)]
        NPS = 8
        ps = [st.enter_context(nc.psum_tensor("ps%d" % i, [128, 512], F32)) for i in range(NPS)]
        r_ps = [Res("ps%d" % i, True) for i in range(NPS)]
        r_h = [Res("h%d" % i) for i in range(2)]
        r_u = [Res("u%d" % i) for i in range(2)]
        r_vec = Res("vec"); r_coef = Res("coef"); r_ones = Res("ones"); r_sq = Res("sq"); r_rstd = Res("rstd")
        r_tmp = [Res("tmp0"), Res("tmp1")]
        if do_main:
            oT = sb("oT_sb", [128, 10, TP], BF16)
            big = sb("big", [128, 24, TP], BF16)
            mrg = sb("mrg", [128, 8, TP], BF16)
            yb = sb("yb", [128, 8, TP], F32)
            NW = 4
            wsl = [sb("wsl%d" % i, [128, 22 * 128], BF16) for i in range(NW)]
            r_w = [Res("w%d" % i) for i in range(NW)]
            r_o = Res("o"); r_big = [Res("big0"), Res("big1")]; r_mrg = [Res("m0"), Res("m1")]; r_y = [Res("y0"), Res("y1")]
        psi = [0]

        def nps():
            psi[0] = (psi[0] + 1) % NPS
            return psi[0]

        wi = [0]

        def loadw(src_block, kc_n):
            wi[0] = (wi[0] + 1) % NW
            k = wi[0]
            S.dma("pool", lambda e: e.dma_start(out=wsl[k][:, 0:kc_n * 128], in_=src_block.rearrange("p k n -> p (k n)")), writes=[r_w[k]])
            return k

        S.dma("sp", lambda e: e.dma_start(out=vec[:], in_=vec_d), writes=[r_vec])
        S.op("pool", lambda e: e.memset(ones[:], 1.0), writes=[r_ones])
        def mkA(i, g, sc):
            S.op("dve", lambda e: e.scalar_tensor_tensor(out=coef[:, i, :], in0=vec[:, sc, :], scalar=1.0, in1=vec[:, g, :], op0=ALU.add, op1=ALU.mult),
                 reads=[r_vec], writes=[r_coef])
            S.op("dve", lambda e: e.tensor_scalar_mul(out=coef[:, i, :], in0=coef[:, i, :], scalar1=32.0), reads=[r_coef], writes=[r_coef])
        def mkG(i, gt, gp):
            S.op("dve", lambda e: e.scalar_tensor_tensor(out=coef[:, i, :], in0=vec[:, gt, :], scalar=32.0, in1=vec[:, gp, :], op0=ALU.mult, op1=ALU.mult),
                 reads=[r_vec], writes=[r_coef])
        def mkB(i, sh):
            S.op("dve", lambda e: e.tensor_copy(out=coef[:, i, :], in_=vec[:, sh, :]), reads=[r_vec], writes=[r_coef])
        mkA(0, 0, 1); mkB(1, 2); mkG(2, 3, 4); mkA(3, 5, 6); mkB(4, 7); mkG(5, 8, 9); mkA(6, 10, 11); mkB(7, 12)

        def rms_stats(src, sc, r_src):
            cs = slice(512 * sc, 512 * sc + 512)
            S.op("act", lambda e: e.activation(out=sq[:], in_=src[:, :, cs], func=AF.Square), reads=[r_src], writes=[r_sq])
            x = nps()
            for kc in range(8):
                S.op("pe", lambda e, kc=kc: e.matmul(ps[x][:], ones[:], sq[:, kc, :], start=(kc == 0), stop=(kc == 7)),
                     reads=[r_ones, r_sq], writes=[r_ps[x]])
            S.op("dve", lambda e: e.tensor_scalar_add(out=rstd[:], in0=ps[x][:], scalar1=1024.0 * EPS), reads=[r_ps[x]], writes=[r_rstd])
            S.op("act", lambda e: e.activation(out=rstd[:], in_=rstd[:], func=AF.Sqrt), reads=[r_rstd], writes=[r_rstd])
            S.op("dve", lambda e: e.reciprocal(out=rstd[:], in_=rstd[:]), reads=[r_rstd], writes=[r_rstd])

        def modulate(sc, ia, ib, dst, r_dst):
            cs = slice(512 * sc, 512 * sc + 512)
            rms_stats(hT, sc, r_h[sc])
            for kc in range(8):
                k = kc % 2
                S.op("dve", lambda e, kc=kc, k=k: e.scalar_tensor_tensor(out=tmp[k][:], in0=hT[:, kc, cs], scalar=coef[:, ia, kc:kc + 1], in1=rstd[:],
                                                                       op0=ALU.mult, op1=ALU.mult),
                     reads=[r_h[sc], r_coef, r_rstd], writes=[r_tmp[k]])
                S.op("act", lambda e, kc=kc, k=k: e.activation(out=dst[:, kc, cs], in_=tmp[k][:], func=AF.Identity, bias=coef[:, ib, kc:kc + 1], scale=1.0),
                     reads=[r_tmp[k], r_coef], writes=[r_dst])

        def residual(sc, ig):
            cs = slice(512 * sc, 512 * sc + 512)
            rms_stats(yb, sc, r_y[sc])
            for kc in range(8):
                k = kc % 2
                S.op("dve", lambda e, kc=kc, k=k: e.scalar_tensor_tensor(out=tmp[k][:], in0=yb[:, kc, cs], scalar=coef[:, ig, kc:kc + 1], in1=rstd[:],
                                                                       op0=ALU.mult, op1=ALU.mult),
                     reads=[r_y[sc], r_coef, r_rstd], writes=[r_tmp[k]])
                S.op("pool", lambda e, kc=kc, k=k: e.tensor_tensor(out=hT[:, kc, cs], in0=hT[:, kc, cs], in1=tmp[k][:], op=ALU.add),
                     reads=[r_tmp[k], r_h[sc]], writes=[r_h[sc]])

        out_toks = []
        for p in range(NPASS):
            S.dma("sp", lambda e, p=p: e.dma_start(out=hT[:], in_=h_d[p]), writes=r_h)
            if do_main:
                S.dma("sp", lambda e, p=p: e.dma_start(out=oT[:], in_=o_d[p]), writes=[r_o])
                for sc in range(2):
                    modulate(sc, 0, 1, uT, r_u[sc])
                for nb in range(24):
                    k = loadw(wg_d[nb], 8)
                    for sc in range(2):
                        cs = slice(512 * sc, 512 * sc + 512)
                        x = nps()
                        for kc in range(8):
                            S.op("pe", lambda e, kc=kc, k=k, cs=cs, x=x: e.matmul(ps[x][:], wsl[k][:, kc * 128:(kc + 1) * 128], uT[:, kc, cs], start=(kc == 0), stop=(kc == 7)),
                                 reads=[r_w[k], r_u[sc]], writes=[r_ps[x]])
                        S.op("act", lambda e, nb=nb, cs=cs, x=x: e.activation(out=big[:, nb, cs], in_=ps[x][:], func=AF.Sigmoid), reads=[r_ps[x]], writes=[r_big[sc]])
                for n in range(8):
                    k = loadw(wbr_d[n], 10)
                    for sc in range(2):
                        cs = slice(512 * sc, 512 * sc + 512)
                        xs = []
                        for (k0, k1) in ((0, 4), (4, 8), (8, 10)):
                            x = nps()
                            xs.append(x)
                            for kc in range(k0, k1):
                                S.op("pe", lambda e, kc=kc, k=k, cs=cs, x=x, k0=k0, k1=k1: e.matmul(ps[x][:], wsl[k][:, kc * 128:(kc + 1) * 128], oT[:, kc, cs],
                                                                                              start=(kc == k0), stop=(kc == k1 - 1)),
                                     reads=[r_w[k], r_o], writes=[r_ps[x]])
                        S.op("dve", lambda e, n=n, cs=cs, x=xs[0]: e.tensor_tensor(out=tmp[0][:], in0=ps[x][:], in1=big[:, n, cs], op=ALU.mult),
                             reads=[r_ps[xs[0]], r_big[sc]], writes=[r_tmp[0]])
                        S.op("dve", lambda e, n=n, cs=cs, x=xs[1]: e.tensor_tensor(out=tmp[1][:], in0=ps[x][:], in1=big[:, 8 + n, cs], op=ALU.mult),
                             reads=[r_ps[xs[1]], r_big[sc]], writes=[r_tmp[1]])
                        S.op("pool", lambda e: e.tensor_tensor(out=tmp[0][:], in0=tmp[0][:], in1=tmp[1][:], op=ALU.add),
                             reads=[r_tmp[1]], writes=[r_tmp[0]])
                        S.op("dve", lambda e, n=n, cs=cs, x=xs[2]: e.tensor_tensor(out=tmp[1][:], in0=ps[x][:], in1=big[:, 16 + n, cs], op=ALU.mult),
                             reads=[r_ps[xs[2]], r_big[sc]], writes=[r_tmp[1]])
                        S.op("pool", lambda e, n=n, cs=cs: e.tensor_tensor(out=mrg[:, n, cs], in0=tmp[0][:], in1=tmp[1][:], op=ALU.add),
                             reads=[r_tmp[0], r_tmp[1]], writes=[r_mrg[sc]])
                for n in range(8):
                    k = loadw(wout_d[n], 8)
                    for sc in range(2):
                        cs = slice(512 * sc, 512 * sc + 512)
                        x = nps()
                        for kc in range(8):
                            S.op("pe", lambda e, kc=kc, k=k, cs=cs, x=x: e.matmul(ps[x][:], wsl[k][:, kc * 128:(kc + 1) * 128], mrg[:, kc, cs], start=(kc == 0), stop=(kc == 7)),
                                 reads=[r_w[k], r_mrg[sc]], writes=[r_ps[x]])
                        S.op("act", lambda e, n=n, cs=cs, x=x: e.activation(out=yb[:, n, cs], in_=ps[x][:], func=AF.Copy), reads=[r_ps[x]], writes=[r_y[sc]])
                for sc in range(2):
                    residual(sc, 2)
                for sc in range(2):
                    modulate(sc, 3, 4, uT, r_u[sc])
                for nb in range(22):
                    kg = loadw(wgate_d[nb], 8)
                    ku = loadw(wup_d[nb], 8)
                    for sc in range(2):
                        cs = slice(512 * sc, 512 * sc + 512)
                        xg = nps()
                        for kc in range(8):
                            S.op("pe", lambda e, kc=kc, kg=kg, cs=cs, xg=xg: e.matmul(ps[xg][:], wsl[kg][:, kc * 128:(kc + 1) * 128], uT[:, kc, cs], start=(kc == 0), stop=(kc == 7)),
                                 reads=[r_w[kg], r_u[sc]], writes=[r_ps[xg]])
                        xu = nps()
                        for kc in range(8):
                            S.op("pe", lambda e, kc=kc, ku=ku, cs=cs, xu=xu: e.matmul(ps[xu][:], wsl[ku][:, kc * 128:(kc + 1) * 128], uT[:, kc, cs], start=(kc == 0), stop=(kc == 7)),
                                 reads=[r_w[ku], r_u[sc]], writes=[r_ps[xu]])
                        kk = nb % 2
                        S.op("act", lambda e, xg=xg, kk=kk: e.activation(out=tmp[kk][:], in_=ps[xg][:], func=AF.Silu), reads=[r_ps[xg]], writes=[r_tmp[kk]])
                        S.op("dve", lambda e, nb=nb, cs=cs, xu=xu, kk=kk: e.tensor_tensor(out=big[:, nb, cs], in0=ps[xu][:], in1=tmp[kk][:], op=ALU.mult),
                             reads=[r_ps[xu], r_tmp[kk]], writes=[r_big[sc]])
                for n in range(8):
                    k = loadw(wdown_d[n], 22)
                    for sc in range(2):
                        cs = slice(512 * sc, 512 * sc + 512)
                        x = nps()
                        for kc in range(22):
                            S.op("pe", lambda e, kc=kc, k=k, cs=cs, x=x: e.matmul(ps[x][:], wsl[k][:, kc * 128:(kc + 1) * 128], big[:, kc, cs], start=(kc == 0), stop=(kc == 21)),
                                 reads=[r_w[k], r_big[sc]], writes=[r_ps[x]])
                        S.op("act", lambda e, n=n, cs=cs, x=x: e.activation(out=yb[:, n, cs], in_=ps[x][:], func=AF.Copy), reads=[r_ps[x]], writes=[r_y[sc]])
                for sc in range(2):
                    residual(sc, 5)
                out_toks.append(S.dma("sp", lambda e, p=p: e.dma_start(out=ho_d[p], in_=hT[:]), reads=r_h))
            else:
                out_toks.append(S.dma("sp", lambda e, p=p: e.dma_start(out=ho_d[p], in_=hT[:]), reads=r_h))
            if do_next:
                for sc in range(2):
                    modulate(sc, 6, 7, uT, r_u[sc])
                out_toks.append(S.dma("sp", lambda e, p=p: e.dma_start(out=uo_d[p], in_=uT[:]), reads=r_u))
        S.emit(out_toks)
    return nc


_CACHE = {}


def _get(name, fn):
    if name not in _CACHE:
        _CACHE[name] = fn()
    return _CACHE[name]


def _run(nc, in_maps):
    return run_bass_kernel_spmd(nc, in_maps, core_ids=list(range(8))).results


def _run_mod(c, w_ada, b_ada):
    nc = _get("mod", build_mod)
    in_maps = []
    for cc in range(8):
        wb = np.zeros((6, 128, 8, 512), np.float32)
        bb = np.zeros((128, 24), np.float32)
        for b in range(6):
            q = 24 * cc + 4 * b
            ll, n0 = q // 48, q % 48
            wb[b] = w_ada[ll][:, n0 * 128:n0 * 128 + 512].reshape(8, 128, 512).transpose(1, 0, 2)
        for n in range(24):
            q = 24 * cc + n
            ll, nn = q // 48, q % 48
            bb[:, n] = b_ada[ll][nn * 128:(nn + 1) * 128]
        in_maps.append(dict(c_col=colvec(c[0]), wada=wb, bada=bb))
    res = _run(nc, in_maps)
    mod = np.zeros((4, 6144), np.float32)
    for cc in range(8):
        for n in range(24):
            q = 24 * cc + n
            ll, nn = q // 48, q % 48
            mod[ll, nn * 128:(nn + 1) * 128] = res[cc]["modc"][:, n]
    return mod


def _make_vecs(P, mod, l, first=False):
    z = np.zeros(1024, np.float32)
    if first:
        vs = [z] * 10 + [P["g_pre_mix"][0], mod[0].reshape(6, 1024)[1], mod[0].reshape(6, 1024)[0]]
    else:
        m = mod[l].reshape(6, 1024)
        if l + 1 < 4:
            mn = mod[l + 1].reshape(6, 1024); gn = P["g_pre_mix"][l + 1]
        else:
            mn = m; gn = P["g_pre_mix"][l]
        vs = [P["g_pre_mix"][l], m[1], m[0], m[2], P["g_post_mix"][l], P["g_pre_ffn"][l], m[4], m[3], m[5], P["g_post_ffn"][l],
              gn, mn[1], mn[0]]
    return np.ascontiguousarray(np.stack([colvec(np.asarray(v, np.float32)) for v in vs], axis=1))


def _to_passes(aT):
    F = aT.shape[0]
    return np.ascontiguousarray(aT.reshape(F // 128, 128, NPASS, TP).transpose(2, 1, 0, 3))


def _from_passes(a):
    kc = a.shape[2]
    return a.transpose(2, 1, 0, 3).reshape(kc * 128, NPASS * TP)


def kernel(x, c, w_ada, b_ada, g_pre_mix, g_post_mix, w_in, sinks, w_br_a, w_br_b, w_br_c,
           w_out, g_pre_ffn, g_post_ffn, w_gate, w_up, w_down):
    P = dict(g_pre_mix=np.asarray(g_pre_mix), g_post_mix=np.asarray(g_post_mix), g_pre_ffn=np.asarray(g_pre_ffn),
             g_post_ffn=np.asarray(g_post_ffn))
    x = np.asarray(x); c = np.asarray(c); w_ada = np.asarray(w_ada); b_ada = np.asarray(b_ada)
    w_in = np.asarray(w_in); sinks = np.asarray(sinks)
    mod = _run_mod(c, w_ada, b_ada)
    xT = np.ascontiguousarray(x[0].T)
    hs = [_to_passes(xT[:, 2048 * cc:2048 * cc + 2048]) for cc in range(8)]
    nc0 = _get("p0", lambda: build_dense(False, True))
    v0 = _make_vecs(P, mod, 0, first=True)
    res = _run(nc0, [dict(hT=hs[cc], vecs=v0) for cc in range(8)])
    us = [res[cc]["uT_out"] for cc in range(8)]
    consts = [att_consts(cc) for cc in range(8)]
    for l in range(4):
        uT = np.ascontiguousarray(np.concatenate([_from_passes(np.asarray(u)) for u in us], axis=1))
        nca = _get("att", build_att)
        in_maps = []
        for cc in range(8):
            m = dict(uT=uT)
            m.update(consts[cc])
            m.update(att_weights(w_in[l], sinks[l], cc))
            in_maps.append(m)
        ra = _run(nca, in_maps)
        oT = np.concatenate([np.asarray(ra[cc]["oa"]) for cc in range(8)] + [np.asarray(ra[cc]["ob"]) for cc in range(8)]
                            + [np.asarray(ra[cc]["oc"]) for cc in range(4)], axis=0)
        last = (l == 3)
        ncd = _get("dense_last" if last else "dense", lambda: build_dense(True, not last))
        wts = dict(w_g=blockW(w_in[l][:, 4608:7680]),
                   w_br=blockW(np.concatenate([np.asarray(w_br_a[l]), np.asarray(w_br_b[l]), np.asarray(w_br_c[l])], axis=0)),
                   w_out=blockW(np.asarray(w_out[l])), w_gate=blockW(np.asarray(w_gate[l])), w_up=blockW(np.asarray(w_up[l])),
                   w_down=blockW(np.asarray(w_down[l])))
        vl = _make_vecs(P, mod, l)
        in_maps = []
        for cc in range(8):
            m = dict(hT=hs[cc], oT=_to_passes(np.ascontiguousarray(oT[:, 2048 * cc:2048 * cc + 2048])), vecs=vl)
            m.update(wts)
            in_maps.append(m)
        rd = _run(ncd, in_maps)
        hs = [np.asarray(rd[cc]["hT_out"]) for cc in range(8)]
        if not last:
            us = [rd[cc]["uT_out"] for cc in range(8)]
    hT = np.concatenate([_from_passes(h) for h in hs], axis=1)
    return np.ascontiguousarray(hT.T)[None].astype(np.float32)
```

```python
from contextlib import ExitStack
import os
import numpy as np
import ml_dtypes
import concourse.bass as bass
import concourse.mybir as mybir
from concourse.bass_utils import run_bass_kernel_spmd

F32 = mybir.dt.float32
BF16 = mybir.dt.bfloat16
AF = mybir.ActivationFunctionType
ALU = mybir.AluOpType
AX = mybir.AxisListType

COMPUTE = ("pe", "act", "dve", "pool")
NDMASEM = 8


class Res:
    __slots__ = ("name", "w", "r", "excl")

    def __init__(self, name, excl=False):
        self.name = name
        self.excl = excl
        self.w = None
        self.r = []


class Sched:
    def __init__(self, nc, stack, dma_queues=("sp", "pool")):
        self.nc = nc
        self.ops = {e: [] for e in ("pe", "act", "dve", "pool", "sp")}
        self.sem = {}
        self.cnt = {}
        for e in COMPUTE:
            self.sem[e] = stack.enter_context(nc.semaphore("s_" + e))
            self.cnt[e] = 0
        self.dsem = {}
        self.dcnt = {}
        self.dnext = {}
        for q in dma_queues:
            self.dsem[q] = [stack.enter_context(nc.semaphore("d_%s%d" % (q, i))) for i in range(NDMASEM)]
            self.dcnt[q] = [0] * NDMASEM
            self.dnext[q] = 0
        self.waited = {}
        self.out_tokens = []

    def _waits(self, eng, deps):
        need = {}
        for d in deps:
            if d is None:
                continue
            semkey, val, prod = d
            if prod == "pe" and eng == "pe":
                continue
            if need.get(semkey, 0) < val:
                need[semkey] = val
        waits = []
        for semkey, val in need.items():
            key = (eng, semkey)
            if self.waited.get(key, 0) < val:
                self.waited[key] = val
                waits.append((semkey, val))
        return waits

    def _semof(self, semkey):
        if semkey[0] == "c":
            return self.sem[semkey[1]]
        return self.dsem[semkey[1]][semkey[2]]

    def _deps(self, reads, writes, extra):
        deps = list(extra)
        for r in reads:
            deps.append(r.w)
            if r.excl:
                deps.extend(r.r)
        for w in writes:
            deps.append(w.w)
            deps.extend(w.r)
        return deps

    def _commit(self, tok, reads, writes):
        for r in reads:
            if r.excl:
                r.r = [tok]
            else:
                r.r.append(tok)
        for w in writes:
            w.w = tok
            w.r = []

    def op(self, eng, fn, reads=(), writes=(), extra=()):
        waits = self._waits(eng, self._deps(reads, writes, extra))
        self.cnt[eng] += 1
        tok = (("c", eng), self.cnt[eng], eng)
        self.ops[eng].append((waits, fn, (("c", eng), 1)))
        self._commit(tok, reads, writes)
        return tok

    def dma(self, q, fn, reads=(), writes=(), extra=()):
        i = self.dnext[q]
        self.dnext[q] = (i + 1) % NDMASEM
        semkey = ("d", q, i)
        deps = self._deps(reads, writes, extra)
        if self.dcnt[q][i] > 0:
            deps.append((semkey, self.dcnt[q][i], "dma"))
        waits = self._waits(q, deps)
        self.dcnt[q][i] += 16
        tok = (semkey, self.dcnt[q][i], "dma")
        self.ops[q].append((waits, fn, (semkey, 16)))
        self._commit(tok, reads, writes)
        return tok

    def emit(self, final_tokens):
        nc = self.nc
        fw = self._waits("sp", final_tokens)
        with nc.Block() as block:
            def run(engname):
                def body(eng):
                    for waits, fn, inc in self.ops[engname]:
                        for semkey, val in waits:
                            eng.wait_ge(self._semof(semkey), val)
                        fn(eng).then_inc(self._semof(inc[0]), inc[1])
                    if engname == "sp":
                        for semkey, val in fw:
                            eng.wait_ge(self._semof(semkey), val)
                return body
            block.sync(run("sp"))
            block.tensor(run("pe"))
            block.scalar(run("act"))
            block.vector(run("dve"))
            block.gpsimd(run("pool"))


S_TOK = 16384
NCH = 32
BIG = 30000.0
NEG = -30000.0


def alibi_slopes_np():
    n = 28
    return (2.0 ** (-8.0 * (np.arange(n) + 1) / n)).astype(np.float64)


def att_consts(c):
    sl = alibi_slopes_np()
    slope_a = sl[20 + c]
    ik = np.arange(128)[:, None]
    didx = np.arange(131)[None, :]
    biasA = (slope_a * (ik - 128.0 * (didx - 3))).astype(np.float32)
    cm = np.zeros((128, 4, 512), np.float32)
    for e in range(4):
        kp = 128 * e + np.arange(128)[:, None]
        qp = np.arange(512)[None, :]
        kb = e // 2
        qb = qp // 256
        cm[:, e, :] = np.where(qb == kb, (kp <= qp), (kb < qb)).astype(np.float32)
    hc = c % 4
    units = [(sl[c], 1, 127), (sl[8 + hc], 1, 128), (sl[12 + hc], 4, 128), (sl[16 + hc], 16, 128)]
    bu = np.zeros((128, 4, 2, 128), np.float32)
    j = np.arange(128)[:, None]
    i = np.arange(128)[None, :]
    for u, (slope, d, ms) in enumerate(units):
        for part in range(2):
            steps = i + 128 - j if part == 0 else i - j
            ok = (steps >= 0) & (steps <= ms)
            bu[:, u, part, :] = np.where(ok, -slope * steps * d, NEG)
    onehot = np.zeros((64, S_TOK), np.float32)
    onehot[np.arange(S_TOK) // 256, np.arange(S_TOK)] = 1.0
    return dict(biasA=biasA, cmask=cm.reshape(128, 2048).astype(ml_dtypes.bfloat16),
                biasU=bu.reshape(128, 1024), onehot=onehot.astype(ml_dtypes.bfloat16),
                ident=np.eye(128, dtype=np.float32))


def att_weights(w_in_l, sinks_l, c):
    hc = c % 4
    def A(part, h): return w_in_l[:, part * 512 + h * 64: part * 512 + h * 64 + 64]
    def Bq(h): return w_in_l[:, 1536 + h * 64: 1536 + h * 64 + 64]
    def Bkv(part, j): return w_in_l[:, 2048 + part * 128 + j * 64: 2048 + part * 128 + j * 64 + 64]
    def C(g, part, h): return w_in_l[:, 2304 + g * 768 + part * 256 + h * 64: 2304 + g * 768 + part * 256 + h * 64 + 64]
    kvh = c // 4
    wfm = np.concatenate([A(1, c), A(0, c),
                          Bq(c), C(0, 0, hc),
                          Bkv(0, kvh), C(0, 1, hc),
                          C(1, 0, hc), C(2, 0, hc),
                          C(1, 1, hc), C(2, 1, hc)], axis=1)
    wv = np.concatenate([A(2, c), Bkv(1, kvh), C(0, 2, hc), C(1, 2, hc), C(2, 2, hc)], axis=1)
    sink = np.full((128, 1), sinks_l[c], np.float32)
    return dict(wfm=np.ascontiguousarray(wfm), wv=np.ascontiguousarray(wv), sink=sink)


def build_att(n_chunks=NCH):
    nc = bass.Bass("TRN2", target_bir_lowering=False)
    D = lambda name, shape, dt, kind="ExternalInput": nc.dram_tensor(name, shape, dt, kind=kind).ap()
    uT = D("uT", [1024, S_TOK], BF16)
    wfm = D("wfm", [1024, 640], F32)
    wv = D("wv", [1024, 320], F32)
    onehot = D("onehot", [64, S_TOK], BF16)
    biasA_d = D("biasA", [128, 131], F32)
    cmask_d = D("cmask", [128, 2048], BF16)
    biasU_d = D("biasU", [128, 1024], F32)
    sink_d = D("sink", [128, 1], F32)
    ident_d = D("ident", [128, 128], F32)
    oa_d = D("oa", [64, S_TOK], BF16, "ExternalOutput")
    ob_d = D("ob", [64, S_TOK], BF16, "ExternalOutput")
    oc_d = D("oc", [64, S_TOK], BF16, "ExternalOutput")

    with ExitStack() as st:
        S = Sched(nc, st)
        sb = lambda name, shape, dt: st.enter_context(nc.sbuf_tensor(name, shape, dt))
        pst = lambda name: st.enter_context(nc.psum_tensor(name, [128, 512], F32))
        KT = sb("KT", [128, S_TOK], BF16)
        VA = sb("VA", [128, 128, 128], BF16)
        usb = sb("usb", [128, 8, 2048], BF16)
        wfm_sb = sb("wfm_sb", [128, 8, 640], BF16)
        wv_sb = sb("wv_sb", [128, 8, 320], BF16)
        Q1 = sb("Q1", [128, 2048], BF16)
        Q23 = sb("Q23", [128, 2048], BF16)
        K1 = [sb("K1_%d" % i, [128, 2048], BF16) for i in range(2)]
        K23 = [sb("K23_%d" % i, [128, 2048], BF16) for i in range(2)]
        VU = [[sb("VU%d_%d" % (u, i), [128, 16, 128], BF16) for i in range(2)] for u in range(4)]
        accC = sb("accC", [128, 2048], F32)
        scb = sb("scb", [128, 2, 512], F32)
        Pb = sb("Pb", [128, 2, 512], BF16)
        PT = [sb("PT%d" % i, [128, 512], BF16) for i in range(2)]
        QTA = [sb("QTA%d" % i, [128, 512], BF16) for i in range(2)]
        QTF = [sb("QTF%d" % i, [64, 512], F32) for i in range(2)]
        ksum = sb("ksum", [64, 64], F32)
        gs = sb("gs", [128, 4, 64], F32)
        top8 = sb("top8", [128, 4, 8], F32)
        Mtok = sb("Mtok", [128, 4, 128], F32)
        biasA = sb("biasA_sb", [128, 131], F32)
        cmask = sb("cmask_sb", [128, 4, 512], BF16)
        biasU = sb("biasU_sb", [128, 4, 2, 128], F32)
        sink = sb("sink_sb", [128, 1], F32)
        esink = sb("esink", [128, 1], F32)
        ident = sb("ident_sb", [128, 128], F32)
        rden = sb("rden", [128, 512], F32)
        oa_sb = [sb("oa_sb%d" % i, [64, 512], BF16) for i in range(2)]
        ob_sb = [sb("ob_sb%d" % i, [64, 512], BF16) for i in range(2)]
        oc_sb = ob_sb
        psP = [pst("psP0"), pst("psP1")]
        psS = [pst("psS0"), pst("psS1")]
        psO = pst("psO")
        psB = [pst("psB0"), pst("psB1")]
        psBO = pst("psBO")

        R = Res
        r_w = R("w"); r_const = R("const"); r_ones = R("ones")
        r_u = [R("u%d" % i) for i in range(4)]
        r_kt = [R("kt%d" % i) for i in range(NCH)]
        r_va = [R("va%d" % i) for i in range(NCH)]
        r_ks = [R("ks%d" % i) for i in range(NCH)]
        r_q1 = [R("q1") for i in range(4)]; r_q23 = [R("q23") for i in range(4)]
        r_k1 = [[R("k1") for i in range(4)] for s in range(2)]
        r_k23 = [[R("k23") for i in range(4)] for s in range(2)]
        r_vu = [[[R("vu") for i in range(4)] for s in range(2)] for u in range(4)]
        r_acc = R("acc"); r_sc = R("sc"); r_Pb = R("Pb")
        r_PT = [R("PT0"), R("PT1")]
        r_qta = [R("qta0"), R("qta1")]; r_qtf = [R("qtf0"), R("qtf1")]
        r_gs = R("gs"); r_top8 = R("top8"); r_M = R("M")
        r_rden = R("rden"); r_oa = [R("oa0"), R("oa1")]; r_ob = [R("ob0"), R("ob1")]
        r_oc = r_ob
        r_psP = [R("psP0", True), R("psP1", True)]; r_psS = [R("psS0", True), R("psS1", True)]; r_psO = R("psO", True)
        r_psB = [R("psB0", True), R("psB1", True)]; r_psBO = R("psBO", True)
        r_esink = R("esink")

        import os
        DBG = int(os.environ.get("ATT_DBG", "99"))
        S.dma("pool", lambda e: e.dma_start(out=wfm_sb[:], in_=wfm.rearrange("(kc p) m -> p kc m", p=128)), writes=[r_w])
        S.dma("pool", lambda e: e.dma_start(out=wv_sb[:], in_=wv.rearrange("(kc p) m -> p kc m", p=128)), writes=[r_w])
        S.dma("sp", lambda e: e.dma_start(out=KT[64:128, :], in_=onehot), writes=[r_const])
        S.dma("sp", lambda e: e.dma_start(out=biasA[:], in_=biasA_d), writes=[r_const])
        S.dma("sp", lambda e: e.dma_start(out=cmask[:], in_=cmask_d.rearrange("p (e q) -> p e q", e=4)), writes=[r_const])
        S.dma("sp", lambda e: e.dma_start(out=biasU[:], in_=biasU_d.rearrange("p (u a q) -> p u a q", u=4, a=2)), writes=[r_const])
        S.dma("sp", lambda e: e.dma_start(out=sink[:], in_=sink_d), writes=[r_const])
        S.dma("sp", lambda e: e.dma_start(out=ident[:], in_=ident_d), writes=[r_const])
        if DBG >= 2:
            S.op("pool", lambda e: e.memset(VA[:, :, 64:128], 1.0), writes=[r_ones])
        for u in range(4 if DBG >= 2 else 0):
            for s in range(2):
                S.op("pool", lambda e, u=u, s=s: e.memset(VU[u][s][:, :, 64:128], 1.0), writes=[r_ones])
        S.op("pool", lambda e: e.memset(ksum[:], 0.0), writes=[r_ks[0]])
        S.op("pool", lambda e: e.memset(Mtok[:], 0.0), writes=[r_M])
        S.op("act", lambda e: e.activation(out=esink[:], in_=sink[:], func=AF.Exp), reads=[r_const], writes=[r_esink])

        out_toks = []
        evq = [0]

        def evac(out, in_, reads, writes):
            return S.op("dve", lambda e: e.tensor_copy(out=out, in_=in_), reads=reads, writes=writes)

        def dve_copy(out, in_, reads, writes):
            return S.op("dve", lambda e: e.tensor_copy(out=out, in_=in_), reads=reads, writes=writes)

        pcount = [0]

        def next_psP():
            pcount[0] += 1
            return pcount[0] % 2

        def proj(t):
            ci = t % 4
            s = t // 4
            ss = s % 2
            off = 512 * ci
            col = 512 * t
            q = "sp" if t % 2 == 0 else "pool"
            S.dma("sp", lambda e: e.dma_start(out=usb[:, :, off:off + 512],
                                              in_=uT.rearrange("(kc p) n -> p kc n", p=128)[:, :, col:col + 512]),
                  writes=[r_u[ci]])
            slot = t % 2
            for mc in range(5 if DBG >= 4 else 0):
                if mc == 0 and DBG < 5:
                    continue
                x = next_psP()
                for kc in range(8):
                    S.op("pe", lambda e, x=x, kc=kc, mc=mc: e.matmul(psP[x][:], wfm_sb[:, kc, mc * 128:(mc + 1) * 128],
                                                                     usb[:, kc, off:off + 512], start=(kc == 0), stop=(kc == 7)),
                         reads=[r_w, r_u[ci]], writes=[r_psP[x]])
                if mc == 0:
                    dve_copy(KT[0:64, col:col + 512], psP[x][0:64, :], [r_psP[x]], [r_kt[t]])
                    dve_copy(QTA[slot][0:64, :], psP[x][64:128, :], [r_psP[x]], [r_qta[slot]])
                    dve_copy(QTF[slot][0:64, :], psP[x][64:128, :], [r_psP[x]], [r_qtf[slot]])
                    S.op("dve", lambda e, x=x: e.tensor_reduce(out=ksum[:, 2 * t:2 * t + 2],
                                                               in_=psP[x][0:64, :].rearrange("p (a b) -> p a b", b=256),
                                                               axis=AX.X, op=ALU.add),
                         reads=[r_psP[x]], writes=[r_ks[t]])
                elif mc == 1:
                    evac(Q1[:, off:off + 512], psP[x][:], [r_psP[x]], [r_q1[ci]])
                elif mc == 2:
                    evac(K1[ss][:, off:off + 512], psP[x][:], [r_psP[x]], [r_k1[ss][ci]])
                elif mc == 3:
                    evac(Q23[:, off:off + 512], psP[x][:], [r_psP[x]], [r_q23[ci]])
                else:
                    evac(K23[ss][:, off:off + 512], psP[x][:], [r_psP[x]], [r_k23[ss][ci]])
                yield
            for p in range(2 if DBG >= 6 else 0):
                x = next_psP()
                for j in range(2):
                    tl = 2 * p + j
                    for kc in range(8):
                        S.op("pe", lambda e, x=x, kc=kc, j=j, tl=tl: e.matmul(
                            psP[x][:, j * 192:(j + 1) * 192], usb[:, kc, off + 128 * tl: off + 128 * tl + 128],
                            wv_sb[:, kc, 0:192], start=(kc == 0), stop=(kc == 7)),
                            reads=[r_w, r_u[ci]], writes=[r_psP[x]])
                pv = psP[x][:, 0:384].rearrange("p (j c) -> p j c", j=2)
                ta = 4 * t + 2 * p
                tb = 4 * ci + 2 * p
                evac(VA[:, ta:ta + 2, 0:64], pv[:, :, 0:64], [r_psP[x]], [r_va[t]])
                evac(VU[0][ss][:, tb:tb + 2, 0:64], pv[:, :, 64:128], [r_psP[x]], [r_vu[0][ss][ci]])
                evac(VU[1][ss][:, tb:tb + 2, 0:64], pv[:, :, 128:192], [r_psP[x]], [r_vu[1][ss][ci]])
                yield

        def maskprep(t):
            slot = t % 2
            x = next_psP()
            nb = 2 * t + 1
            for qt in range(4):
                S.op("pe", lambda e, x=x, qt=qt: e.matmul(psP[x][:, qt * 64:(qt + 1) * 64], QTF[slot][0:64, qt * 128:(qt + 1) * 128],
                                                          ksum[:, :], start=True, stop=True),
                     reads=[r_qtf[slot]] + r_ks[0:t + 1], writes=[r_psP[x]])
            S.op("dve", lambda e, x=x: e.tensor_copy(out=gs[:], in_=psP[x][:, 0:256].rearrange("p (a b) -> p a b", b=64)),
                 reads=[r_psP[x]], writes=[r_gs])
            yield
            for half in range(2):
                b = 2 * t + half
                qs = slice(2 * half, 2 * half + 2)
                if b >= 4:
                    if b < 8:
                        S.op("dve", lambda e, b=b, qs=qs: e.memset(gs[:, qs, b:8], -1e30), reads=[], writes=[r_gs])
                    w = max(b, 8)
                    for qt in range(2 * half, 2 * half + 2):
                        S.op("dve", lambda e, qt=qt, w=w: e.max(out=top8[:, qt, :], in_=gs[:, qt, 0:w]), reads=[r_gs], writes=[r_top8])
                        S.op("dve", lambda e, qt=qt, b=b: e.tensor_scalar(out=Mtok[:, qt, 64:64 + b], in0=gs[:, qt, 0:b],
                                                                          scalar1=top8[:, qt, 2:3], scalar2=BIG,
                                                                          op0=ALU.is_ge, op1=ALU.mult),
                             reads=[r_gs, r_top8], writes=[r_M])
                elif b > 0:
                    S.op("dve", lambda e, qs=qs, b=b: e.memset(Mtok[:, qs, 64:64 + b], BIG), writes=[r_M])
                S.op("dve", lambda e, qs=qs, b=b: e.memset(Mtok[:, qs, 64 + b:64 + b + 1], BIG), writes=[r_M])
                if b + 1 < 64:
                    S.op("dve", lambda e, qs=qs, b=b: e.memset(Mtok[:, qs, 64 + b + 1:128], 0.0), writes=[r_M])
            yield
            y = next_psP()
            for qt in range(4):
                S.op("pe", lambda e, y=y, qt=qt: e.matmul(psP[y][:, qt * 128:(qt + 1) * 128], Mtok[:, qt, :], ident[:], start=True, stop=True),
                     reads=[r_M, r_const], writes=[r_psP[y]])
            S.op("dve", lambda e, y=y: e.tensor_scalar_add(out=QTA[slot][64:128, :], in0=psP[y][64:128, :], scalar1=-BIG),
                 reads=[r_psP[y]], writes=[r_qta[slot]])
            yield

        def moba(t, bg):
            slot = t % 2
            nkt = 4 * t + 4
            def pv(kt):
                p = kt % 2
                S.op("pe", lambda e: e.matmul(psO[:], VA[:, kt, :], PT[p][:], start=(kt == 0), stop=(kt == nkt - 1)),
                     reads=[r_va[kt // 4], r_PT[p], r_ones], writes=[r_psO])
            for kt in range(nkt):
                p = kt % 2
                S.op("pe", lambda e, kt=kt, p=p: e.matmul(psS[p][:], KT[:, kt * 128:(kt + 1) * 128], QTA[slot][:], start=True, stop=True),
                     reads=[r_kt[kt // 4], r_const, r_qta[slot]], writes=[r_psS[p]])
                di = 4 * t - kt + 3
                S.op("act", lambda e, p=p, di=di: e.activation(out=PT[p][:], in_=psS[p][:], func=AF.Exp, bias=biasA[:, di:di + 1], scale=0.125),
                     reads=[r_psS[p], r_const], writes=[r_PT[p]])
                if kt >= 4 * t:
                    ee = kt - 4 * t
                    S.op("dve", lambda e, p=p, ee=ee: e.tensor_tensor(out=PT[p][:], in0=PT[p][:], in1=cmask[:, ee, :], op=ALU.mult),
                         reads=[r_const, r_PT[p]], writes=[r_PT[p]])
                if kt >= 1:
                    pv(kt - 1)
                next(bg, None)
                if t < 8:
                    next(bg, None)
            pv(nkt - 1)
            S.op("dve", lambda e: e.reciprocal(out=rden[64:128, :], in_=psO[64:128, :]), reads=[r_psO], writes=[r_rden])
            S.op("dve", lambda e: e.tensor_tensor(out=oa_sb[slot][:], in0=psO[0:64, :], in1=rden[64:128, :], op=ALU.mult),
                 reads=[r_psO, r_rden], writes=[r_oa[slot]])
            out_toks.append(S.dma("sp", lambda e: e.dma_start(out=oa_d[:, 512 * t:512 * t + 512], in_=oa_sb[slot][:]), reads=[r_oa[slot]]))

        def tile_cols(d, idx):
            if d == 1:
                return slice(128 * idx, 128 * idx + 128)
            if d == 4:
                r, m = idx // 4, idx % 4
                return slice(512 * m + r, 512 * m + 512, 4)
            return slice(idx, 2048, 16)

        def cis(d, idx):
            if d == 1:
                return [idx // 4]
            if d == 4:
                return [idx % 4]
            return [0, 1, 2, 3]

        def strided_v(s):
            ss = s % 2
            for u, d, wc in ((2, 4, 192), (3, 16, 256)):
                for half in range(2):
                    x = next_psP()
                    for j in range(8):
                        idx = half * 8 + j
                        cs = tile_cols(d, idx)
                        for kc in range(8):
                            S.op("pe", lambda e, x=x, j=j, kc=kc, cs=cs, wc=wc: e.matmul(
                                psP[x][:, j * 64:(j + 1) * 64], usb[:, kc, cs], wv_sb[:, kc, wc:wc + 64],
                                start=(kc == 0), stop=(kc == 7)), reads=[r_w] + r_u, writes=[r_psP[x]])
                    evac(VU[u][ss][:, half * 8:half * 8 + 8, 0:64], psP[x][:].rearrange("p (j c) -> p j c", c=64),
                         [r_psP[x]], r_vu[u][ss])
                    yield

        UNITS = [(0, 1, Q1, K1, 0, r_q1, r_k1), (1, 1, Q1, K1, 64, r_q1, r_k1),
                 (2, 4, Q23, K23, 0, r_q23, r_k23), (3, 16, Q23, K23, 64, r_q23, r_k23)]

        def do_quad(s, unit, a):
            (u, d, Qb, Kb, rb, rq, rk) = unit
            ss = s % 2
            ps_ = 1 - ss
            rows = slice(rb, rb + 64)
            quad = [4 * a + j for j in range(4)]
            prevs = []
            for idx in quad:
                if d == 1:
                    pr = (ss, idx - 1) if idx > 0 else ((ps_, 15) if s > 0 else None)
                elif d == 4:
                    pr = (ss, idx - 1) if idx % 4 > 0 else ((ps_, idx + 3) if s > 0 else None)
                else:
                    pr = (ps_, idx) if s > 0 else None
                prevs.append(pr)
            anyprev = any(p is not None for p in prevs)
            for j, idx in enumerate(quad):
                cs = tile_cols(d, idx)
                rq_l = [rq[i] for i in cis(d, idx)]
                if prevs[j] is not None:
                    psl, pidx = prevs[j]
                    pcs = tile_cols(d, pidx)
                    S.op("pe", lambda e, j=j, psl=psl, pcs=pcs, cs=cs: e.matmul(psB[0][:, 128 * j:128 * j + 128], Kb[psl][rows, pcs], Qb[rows, cs], start=True, stop=True),
                         reads=rq_l + [rk[psl][i] for i in cis(d, pidx)], writes=[r_psB[0]])
                S.op("pe", lambda e, j=j, cs=cs: e.matmul(psB[1][:, 128 * j:128 * j + 128], Kb[ss][rows, cs], Qb[rows, cs], start=True, stop=True),
                     reads=rq_l + [rk[ss][i] for i in cis(d, idx)], writes=[r_psB[1]])
            j0 = min([j for j in range(4) if prevs[j] is not None], default=4)
            S.op("dve", lambda e: e.scalar_tensor_tensor(
                out=scb[:, 1, :].rearrange("p (j q) -> p j q", j=4),
                in0=psB[1][:].rearrange("p (j q) -> p j q", j=4), scalar=0.125,
                in1=biasU[:, u, 1:2, :].to_broadcast([128, 4, 128]),
                op0=ALU.mult, op1=ALU.add), reads=[r_psB[1], r_const], writes=[r_sc])
            if j0 < 4:
                S.op("dve", lambda e: e.scalar_tensor_tensor(
                    out=scb[:, 0, 128 * j0:512].rearrange("p (j q) -> p j q", q=128),
                    in0=psB[0][:, 128 * j0:512].rearrange("p (j q) -> p j q", q=128), scalar=0.125,
                    in1=biasU[:, u, 0:1, :].to_broadcast([128, 4 - j0, 128]),
                    op0=ALU.mult, op1=ALU.add), reads=[r_psB[0], r_const], writes=[r_sc])
            if j0 == 0:
                S.op("act", lambda e: e.activation(out=Pb[:], in_=scb[:], func=AF.Exp), reads=[r_sc], writes=[r_Pb])
            else:
                S.op("act", lambda e: e.activation(out=Pb[:, 1, :], in_=scb[:, 1, :], func=AF.Exp), reads=[r_sc], writes=[r_Pb])
                if j0 < 4:
                    S.op("act", lambda e: e.activation(out=Pb[:, 0, 128 * j0:512], in_=scb[:, 0, 128 * j0:512], func=AF.Exp), reads=[r_sc], writes=[r_Pb])
            for j, idx in enumerate(quad):
                has_prev = prevs[j] is not None
                if has_prev:
                    psl, pidx = prevs[j]
                    S.op("pe", lambda e, j=j, psl=psl, pidx=pidx: e.matmul(psBO[:, 128 * j:128 * j + 128], VU[u][psl][:, pidx, :], Pb[:, 0, 128 * j:128 * j + 128], start=True, stop=False),
                         reads=[r_Pb, r_ones] + r_vu[u][psl], writes=[r_psBO])
                S.op("pe", lambda e, j=j, idx=idx, has_prev=has_prev: e.matmul(psBO[:, 128 * j:128 * j + 128], VU[u][ss][:, idx, :], Pb[:, 1, 128 * j:128 * j + 128], start=(not has_prev), stop=True),
                     reads=[r_Pb, r_ones] + r_vu[u][ss], writes=[r_psBO])
            if u == 0:
                bs = (4 * s + a) % 2
                S.op("dve", lambda e: e.tensor_scalar_add(out=rden[64:128, :], in0=psBO[64:128, :], scalar1=esink[64:128, 0:1]),
                     reads=[r_psBO, r_esink], writes=[r_rden])
                S.op("dve", lambda e: e.reciprocal(out=rden[64:128, :], in_=rden[64:128, :]), reads=[r_rden], writes=[r_rden])
                S.op("dve", lambda e: e.tensor_tensor(out=ob_sb[bs][:], in0=psBO[0:64, :], in1=rden[64:128, :], op=ALU.mult),
                     reads=[r_psBO, r_rden], writes=[r_ob[bs]])
                c0 = 2048 * s + 512 * a
                out_toks.append(S.dma("sp", lambda e: e.dma_start(out=ob_d[:, c0:c0 + 512], in_=ob_sb[bs][:]), reads=[r_ob[bs]]))
            elif u == 1:
                dve_copy(accC[:, 512 * a:512 * a + 512], psBO[:], [r_psBO], [r_acc])
            elif u == 2:
                av = accC[:].rearrange("p (m i r) -> p m i r", m=4, i=128, r=4)[:, :, :, a]
                S.op("dve", lambda e: e.tensor_tensor(out=av, in0=av, in1=psBO[:].rearrange("p (m i) -> p m i", m=4), op=ALU.add),
                     reads=[r_psBO, r_acc], writes=[r_acc])
            else:
                av = accC[:].rearrange("p (i r) -> p i r", r=16)[:, :, 4 * a:4 * a + 4]
                S.op("dve", lambda e: e.tensor_tensor(out=av, in0=av, in1=psBO[:].rearrange("p (j i) -> p i j", j=4), op=ALU.add),
                     reads=[r_psBO, r_acc], writes=[r_acc])

        def finish_c(s):
            for a in range(4):
                bs = a % 2
                S.op("dve", lambda e, a=a: e.reciprocal(out=rden[0:64, :], in_=accC[64:128, 512 * a:512 * a + 512]), reads=[r_acc], writes=[r_rden])
                S.op("dve", lambda e, a=a, bs=bs: e.tensor_tensor(out=oc_sb[bs][:], in0=accC[0:64, 512 * a:512 * a + 512], in1=rden[0:64, :], op=ALU.mult),
                     reads=[r_acc, r_rden], writes=[r_oc[bs]])
                c0 = 2048 * s + 512 * a
                out_toks.append(S.dma("sp", lambda e, bs=bs, c0=c0: e.dma_start(out=oc_d[:, c0:c0 + 512], in_=oc_sb[bs][:]), reads=[r_oc[bs]]))

        def banded(s):
            for unit in UNITS:
                for a in range(4):
                    do_quad(s, unit, a)
                    yield
            finish_c(s)
            yield

        import os
        parts = os.environ.get("ATT_PARTS", "mask,moba,band").split(",")
        import itertools
        def drain(g):
            for _ in g:
                pass
        drain(proj(0)); drain(maskprep(0))
        for t in range(n_chunks):
            gens = []
            if t % 4 == 3:
                gens += [strided_v(t // 4), banded(t // 4)]
            if t + 1 < n_chunks:
                gens += [proj(t + 1), maskprep(t + 1)]
            bg = itertools.chain(*gens)
            moba(t, bg)
            drain(bg)
        if not out_toks:
            out_toks.append(S.dma("sp", lambda e: e.dma_start(out=oa_d[:, 0:512], in_=QTA[0][0:64, :]), reads=[r_qta[0]]))
        S.emit(out_toks)
    return nc


TP = 1024
NPASS = 2
EPS = 1e-6
D_FF = 2816
NV = 13


def blockW(w, kc_rows=None):
    K, N = w.shape
    return np.ascontiguousarray(w.reshape(K // 128, 128, N // 128, 128).transpose(2, 1, 0, 3))


def colvec(v):
    return np.ascontiguousarray(v.reshape(-1, 128).T)


def build_mod():
    nc = bass.Bass("TRN2", target_bir_lowering=False)
    D = lambda name, shape, dt, kind="ExternalInput": nc.dram_tensor(name, shape, dt, kind=kind).ap()
    c_d = D("c_col", [128, 8], F32)
    w_d = D("wada", [6, 128, 8, 512], F32)
    b_d = D("bada", [128, 24], F32)
    o_d = D("modc", [128, 24], F32, "ExternalOutput")
    with ExitStack() as st:
        S = Sched(nc, st)
        sb = lambda name, shape, dt: st.enter_context(nc.sbuf_tensor(name, shape, dt))
        cc = sb("cc", [128, 8], F32)
        scc = sb("scc", [128, 8], F32)
        bb = sb("bb", [128, 24], F32)
        oo = sb("oo", [128, 24], F32)
        wb = [sb("wb%d" % i, [128, 8, 512], F32) for i in range(2)]
        ps = st.enter_context(nc.psum_tensor("ps", [128, 512], F32))
        r_c = Res("c"); r_sc = Res("sc"); r_b = Res("b"); r_o = Res("o"); r_ps = Res("ps", True)
        r_w = [Res("w0"), Res("w1")]
        S.dma("sp", lambda e: e.dma_start(out=cc[:], in_=c_d), writes=[r_c])
        S.dma("sp", lambda e: e.dma_start(out=bb[:], in_=b_d), writes=[r_b])
        S.op("act", lambda e: e.activation(out=scc[:], in_=cc[:], func=AF.Silu), reads=[r_c], writes=[r_sc])
        for blk in range(6):
            s = blk % 2
            S.dma("sp" if blk % 2 == 0 else "pool", lambda e, blk=blk, s=s: e.dma_start(out=wb[s][:], in_=w_d[blk]), writes=[r_w[s]])
            for j in range(4):
                n = blk * 4 + j
                for kc in range(8):
                    S.op("pe", lambda e, s=s, j=j, kc=kc, n=n: e.matmul(ps[:, n:n + 1], wb[s][:, kc, j * 128:(j + 1) * 128], scc[:, kc:kc + 1],
                                                                        start=(kc == 0), stop=(kc == 7)),
                         reads=[r_w[s], r_sc], writes=[r_ps])
        S.op("dve", lambda e: e.tensor_tensor(out=oo[:], in0=ps[:, 0:24], in1=bb[:], op=ALU.add), reads=[r_ps, r_b], writes=[r_o])
        t = S.dma("sp", lambda e: e.dma_start(out=o_d, in_=oo[:]), reads=[r_o])
        S.emit([t])
    return nc


def build_dense(do_main=True, do_next=True):
    nc = bass.Bass("TRN2", target_bir_lowering=False)
    D = lambda name, shape, dt, kind="ExternalInput": nc.dram_tensor(name, shape, dt, kind=kind).ap()
    h_d = D("hT", [NPASS, 128, 8, TP], F32)
    vec_d = D("vecs", [128, NV, 8], F32)
    ho_d = D("hT_out", [NPASS, 128, 8, TP], F32, "ExternalOutput")
    if do_next:
        uo_d = D("uT_out", [NPASS, 128, 8, TP], BF16, "ExternalOutput")
    if do_main:
        o_d = D("oT", [NPASS, 128, 10, TP], BF16)
        wg_d = D("w_g", [24, 128, 8, 128], F32)
        wbr_d = D("w_br", [8, 128, 10, 128], F32)
        wout_d = D("w_out", [8, 128, 8, 128], F32)
        wgate_d = D("w_gate", [22, 128, 8, 128], F32)
        wup_d = D("w_up", [22, 128, 8, 128], F32)
        wdown_d = D("w_down", [8, 128, 22, 128], F32)

    with ExitStack() as st:
        S = Sched(nc, st)
        sb = lambda name, shape, dt: st.enter_context(nc.sbuf_tensor(name, shape, dt))
        hT = sb("hT_sb", [128, 8, TP], F32)
        uT = sb("uT_sb", [128, 8, TP], BF16)
        vec = sb("vec", [128, NV, 8], F32)
        coef = sb("coef", [128, 8, 8], F32)
        ones = sb("ones", [128, 128], BF16)
        sq = sb("sq", [128, 8, 512], BF16)
        rstd = sb("rstd", [128, 512], F32)
        tmp = [sb("tmp%d" % i, [128, 512], F32) for i in range(2)]
        NPS = 8
        ps = [st.enter_context(nc.psum_tensor("ps%d" % i, [128, 512], F32)) for i in range(NPS)]
        r_ps = [Res("ps%d" % i, True) for i in range(NPS)]
        r_h = [Res("h%d" % i) for i in range(2)]
        r_u = [Res("u%d" % i) for i in range(2)]
        r_vec = Res("vec"); r_coef = Res("coef"); r_ones = Res("ones"); r_sq = Res("sq"); r_rstd = Res("rstd")
        r_tmp = [Res("tmp0"), Res("tmp1")]
        if do_main:
            oT = sb("oT_sb", [128, 10, TP], BF16)
            big = sb("big", [128, 24, TP], BF16)
            mrg = sb("mrg", [128, 8, TP], BF16)
            yb = sb("yb", [128, 8, TP], F32)
            NW = 4
            wsl = [sb("wsl%d" % i, [128, 22 * 128], BF16) for i in range(NW)]
            r_w = [Res("w%d" % i) for i in range(NW)]
            r_o = Res("o"); r_big = [Res("big0"), Res("big1")]; r_mrg = [Res("m0"), Res("m1")]; r_y = [Res("y0"), Res("y1")]
        psi = [0]

        def nps():
            psi[0] = (psi[0] + 1) % NPS
            return psi[0]

        wi = [0]

        def loadw(src_block, kc_n):
            wi[0] = (wi[0] + 1) % NW
            k = wi[0]
            S.dma("pool", lambda e: e.dma_start(out=wsl[k][:, 0:kc_n * 128], in_=src_block.rearrange("p k n -> p (k n)")), writes=[r_w[k]])
            return k

        S.dma("sp", lambda e: e.dma_start(out=vec[:], in_=vec_d), writes=[r_vec])
        S.op("pool", lambda e: e.memset(ones[:], 1.0), writes=[r_ones])
        def mkA(i, g, sc):
            S.op("dve", lambda e: e.scalar_tensor_tensor(out=coef[:, i, :], in0=vec[:, sc, :], scalar=1.0, in1=vec[:, g, :], op0=ALU.add, op1=ALU.mult),
                 reads=[r_vec], writes=[r_coef])
            S.op("dve", lambda e: e.tensor_scalar_mul(out=coef[:, i, :], in0=coef[:, i, :], scalar1=32.0), reads=[r_coef], writes=[r_coef])
        def mkG(i, gt, gp):
            S.op("dve", lambda e: e.scalar_tensor_tensor(out=coef[:, i, :], in0=vec[:, gt, :], scalar=32.0, in1=vec[:, gp, :], op0=ALU.mult, op1=ALU.mult),
                 reads=[r_vec], writes=[r_coef])
        def mkB(i, sh):
            S.op("dve", lambda e: e.tensor_copy(out=coef[:, i, :], in_=vec[:, sh, :]), reads=[r_vec], writes=[r_coef])
        mkA(0, 0, 1); mkB(1, 2); mkG(2, 3, 4); mkA(3, 5, 6); mkB(4, 7); mkG(5, 8, 9); mkA(6, 10, 11); mkB(7, 12)

        def rms_stats(src, sc, r_src):
            cs = slice(512 * sc, 512 * sc + 512)
            S.op("act", lambda e: e.activation(out=sq[:], in_=src[:, :, cs], func=AF.Square), reads=[r_src], writes=[r_sq])
            x = nps()
            for kc in range(8):
                S.op("pe", lambda e, kc=kc: e.matmul(ps[x][:], ones[:], sq[:, kc, :], start=(kc == 0), stop=(kc == 7)),
                     reads=[r_ones, r_sq], writes=[r_ps[x]])
            S.op("dve", lambda e: e.tensor_scalar_add(out=rstd[:], in0=ps[x][:], scalar1=1024.0 * EPS), reads=[r_ps[x]], writes=[r_rstd])
            S.op("act", lambda e: e.activation(out=rstd[:], in_=rstd[:], func=AF.Sqrt), reads=[r_rstd], writes=[r_rstd])
            S.op("dve", lambda e: e.reciprocal(out=rstd[:], in_=rstd[:]), reads=[r_rstd], writes=[r_rstd])

        def modulate(sc, ia, ib, dst, r_dst):
            cs = slice(512 * sc, 512 * sc + 512)
            rms_stats(hT, sc, r_h[sc])
            for kc in range(8):
                k = kc % 2
                S.op("dve", lambda e, kc=kc, k=k: e.scalar_tensor_tensor(out=tmp[k][:], in0=hT[:, kc, cs], scalar=coef[:, ia, kc:kc + 1], in1=rstd[:],
                                                                       op0=ALU.mult, op1=ALU.mult),
                     reads=[r_h[sc], r_coef, r_rstd], writes=[r_tmp[k]])
                S.op("act", lambda e, kc=kc, k=k: e.activation(out=dst[:, kc, cs], in_=tmp[k][:], func=AF.Identity, bias=coef[:, ib, kc:kc + 1], scale=1.0),
                     reads=[r_tmp[k], r_coef], writes=[r_dst])

        def residual(sc, ig):
            cs = slice(512 * sc, 512 * sc + 512)
            rms_stats(yb, sc, r_y[sc])
            for kc in range(8):
                k = kc % 2
                S.op("dve", lambda e, kc=kc, k=k: e.scalar_tensor_tensor(out=tmp[k][:], in0=yb[:, kc, cs], scalar=coef[:, ig, kc:kc + 1], in1=rstd[:],
                                                                       op0=ALU.mult, op1=ALU.mult),
                     reads=[r_y[sc], r_coef, r_rstd], writes=[r_tmp[k]])
                S.op("pool", lambda e, kc=kc, k=k: e.tensor_tensor(out=hT[:, kc, cs], in0=hT[:, kc, cs], in1=tmp[k][:], op=ALU.add),
                     reads=[r_tmp[k], r_h[sc]], writes=[r_h[sc]])

        out_toks = []
        for p in range(NPASS):
            S.dma("sp", lambda e, p=p: e.dma_start(out=hT[:], in_=h_d[p]), writes=r_h)
            if do_main:
                S.dma("sp", lambda e, p=p: e.dma_start(out=oT[:], in_=o_d[p]), writes=[r_o])
                for sc in range(2):
                    modulate(sc, 0, 1, uT, r_u[sc])
                for nb in range(24):
                    k = loadw(wg_d[nb], 8)
                    for sc in range(2):
                        cs = slice(512 * sc, 512 * sc + 512)
                        x = nps()
                        for kc in range(8):
                            S.op("pe", lambda e, kc=kc, k=k, cs=cs, x=x: e.matmul(ps[x][:], wsl[k][:, kc * 128:(kc + 1) * 128], uT[:, kc, cs], start=(kc == 0), stop=(kc == 7)),
                                 reads=[r_w[k], r_u[sc]], writes=[r_ps[x]])
                        S.op("act", lambda e, nb=nb, cs=cs, x=x: e.activation(out=big[:, nb, cs], in_=ps[x][:], func=AF.Sigmoid), reads=[r_ps[x]], writes=[r_big[sc]])
                for n in range(8):
                    k = loadw(wbr_d[n], 10)
                    for sc in range(2):
                        cs = slice(512 * sc, 512 * sc + 512)
                        xs = []
                        for (k0, k1) in ((0, 4), (4, 8), (8, 10)):
                            x = nps()
                            xs.append(x)
                            for kc in range(k0, k1):
                                S.op("pe", lambda e, kc=kc, k=k, cs=cs, x=x, k0=k0, k1=k1: e.matmul(ps[x][:], wsl[k][:, kc * 128:(kc + 1) * 128], oT[:, kc, cs],
                                                                                              start=(kc == k0), stop=(kc == k1 - 1)),
                                     reads=[r_w[k], r_o], writes=[r_ps[x]])
                        S.op("dve", lambda e, n=n, cs=cs, x=xs[0]: e.tensor_tensor(out=tmp[0][:], in0=ps[x][:], in1=big[:, n, cs], op=ALU.mult),
                             reads=[r_ps[xs[0]], r_big[sc]], writes=[r_tmp[0]])
                        S.op("dve", lambda e, n=n, cs=cs, x=xs[1]: e.tensor_tensor(out=tmp[1][:], in0=ps[x][:], in1=big[:, 8 + n, cs], op=ALU.mult),
                             reads=[r_ps[xs[1]], r_big[sc]], writes=[r_tmp[1]])
                        S.op("pool", lambda e: e.tensor_tensor(out=tmp[0][:], in0=tmp[0][:], in1=tmp[1][:], op=ALU.add),
                             reads=[r_tmp[1]], writes=[r_tmp[0]])
                        S.op("dve", lambda e, n=n, cs=cs, x=xs[2]: e.tensor_tensor(out=tmp[1][:], in0=ps[x][:], in1=big[:, 16 + n, cs], op=ALU.mult),
                             reads=[r_ps[xs[2]], r_big[sc]], writes=[r_tmp[1]])
                        S.op("pool", lambda e, n=n, cs=cs: e.tensor_tensor(out=mrg[:, n, cs], in0=tmp[0][:], in1=tmp[1][:], op=ALU.add),
                             reads=[r_tmp[0], r_tmp[1]], writes=[r_mrg[sc]])
                for n in range(8):
                    k = loadw(wout_d[n], 8)
                    for sc in range(2):
                        cs = slice(512 * sc, 512 * sc + 512)
                        x = nps()
                        for kc in range(8):
                            S.op("pe", lambda e, kc=kc, k=k, cs=cs, x=x: e.matmul(ps[x][:], wsl[k][:, kc * 128:(kc + 1) * 128], mrg[:, kc, cs], start=(kc == 0), stop=(kc == 7)),
                                 reads=[r_w[k], r_mrg[sc]], writes=[r_ps[x]])
                        S.op("act", lambda e, n=n, cs=cs, x=x: e.activation(out=yb[:, n, cs], in_=ps[x][:], func=AF.Copy), reads=[r_ps[x]], writes=[r_y[sc]])
                for sc in range(2):
                    residual(sc, 2)
                for sc in range(2):
                    modulate(sc, 3, 4, uT, r_u[sc])
                for nb in range(22):
                    kg = loadw(wgate_d[nb], 8)
                    ku = loadw(wup_d[nb], 8)
                    for sc in range(2):
                        cs = slice(512 * sc, 512 * sc + 512)
                        xg = nps()
                        for kc in range(8):
                            S.op("pe", lambda e, kc=kc, kg=kg, cs=cs, xg=xg: e.matmul(ps[xg][:], wsl[kg][:, kc * 128:(kc + 1) * 128], uT[:, kc, cs], start=(kc == 0), stop=(kc == 7)),
                                 reads=[r_w[kg], r_u[sc]], writes=[r_ps[xg]])
                        xu = nps()
                        for kc in range(8):
                            S.op("pe", lambda e, kc=kc, ku=ku, cs=cs, xu=xu: e.matmul(ps[xu][:], wsl[ku][:, kc * 128:(kc + 1) * 128], uT[:, kc, cs], start=(kc == 0), stop=(kc == 7)),
                                 reads=[r_w[ku], r_u[sc]], writes=[r_ps[xu]])
                        kk = nb % 2
                        S.op("act", lambda e, xg=xg, kk=kk: e.activation(out=tmp[kk][:], in_=ps[xg][:], func=AF.Silu), reads=[r_ps[xg]], writes=[r_tmp[kk]])
                        S.op("dve", lambda e, nb=nb, cs=cs, xu=xu, kk=kk: e.tensor_tensor(out=big[:, nb, cs], in0=ps[xu][:], in1=tmp[kk][:], op=ALU.mult),
                             reads=[r_ps[xu], r_tmp[kk]], writes=[r_big[sc]])
                for n in range(8):
                    k = loadw(wdown_d[n], 22)
                    for sc in range(2):
                        cs = slice(512 * sc, 512 * sc + 512)
                        x = nps()
                        for kc in range(22):
                            S.op("pe", lambda e, kc=kc, k=k, cs=cs, x=x: e.matmul(ps[x][:], wsl[k][:, kc * 128:(kc + 1) * 128], big[:, kc, cs], start=(kc == 0), stop=(kc == 21)),
                                 reads=[r_w[k], r_big[sc]], writes=[r_ps[x]])
                        S.op("act", lambda e, n=n, cs=cs, x=x: e.activation(out=yb[:, n, cs], in_=ps[x][:], func=AF.Copy), reads=[r_ps[x]], writes=[r_y[sc]])
                for sc in range(2):
                    residual(sc, 5)
                out_toks.append(S.dma("sp", lambda e, p=p: e.dma_start(out=ho_d[p], in_=hT[:]), reads=r_h))
            else:
                out_toks.append(S.dma("sp", lambda e, p=p: e.dma_start(out=ho_d[p], in_=hT[:]), reads=r_h))
            if do_next:
                for sc in range(2):
                    modulate(sc, 6, 7, uT, r_u[sc])
                out_toks.append(S.dma("sp", lambda e, p=p: e.dma_start(out=uo_d[p], in_=uT[:]), reads=r_u))
        S.emit(out_toks)
    return nc


_CACHE = {}


def _get(name, fn):
    if name not in _CACHE:
        _CACHE[name] = fn()
    return _CACHE[name]


def _run(nc, in_maps):
    return run_bass_kernel_spmd(nc, in_maps, core_ids=list(range(8))).results


def _run_mod(c, w_ada, b_ada):
    nc = _get("mod", build_mod)
    in_maps = []
    for cc in range(8):
        wb = np.zeros((6, 128, 8, 512), np.float32)
        bb = np.zeros((128, 24), np.float32)
        for b in range(6):
            q = 24 * cc + 4 * b
            ll, n0 = q // 48, q % 48
            wb[b] = w_ada[ll][:, n0 * 128:n0 * 128 + 512].reshape(8, 128, 512).transpose(1, 0, 2)
        for n in range(24):
            q = 24 * cc + n
            ll, nn = q // 48, q % 48
            bb[:, n] = b_ada[ll][nn * 128:(nn + 1) * 128]
        in_maps.append(dict(c_col=colvec(c[0]), wada=wb, bada=bb))
    res = _run(nc, in_maps)
    mod = np.zeros((4, 6144), np.float32)
    for cc in range(8):
        for n in range(24):
            q = 24 * cc + n
            ll, nn = q // 48, q % 48
            mod[ll, nn * 128:(nn + 1) * 128] = res[cc]["modc"][:, n]
    return mod


def _make_vecs(P, mod, l, first=False):
    z = np.zeros(1024, np.float32)
    if first:
        vs = [z] * 10 + [P["g_pre_mix"][0], mod[0].reshape(6, 1024)[1], mod[0].reshape(6, 1024)[0]]
    else:
        m = mod[l].reshape(6, 1024)
        if l + 1 < 4:
            mn = mod[l + 1].reshape(6, 1024); gn = P["g_pre_mix"][l + 1]
        else:
            mn = m; gn = P["g_pre_mix"][l]
        vs = [P["g_pre_mix"][l], m[1], m[0], m[2], P["g_post_mix"][l], P["g_pre_ffn"][l], m[4], m[3], m[5], P["g_post_ffn"][l],
              gn, mn[1], mn[0]]
    return np.ascontiguousarray(np.stack([colvec(np.asarray(v, np.float32)) for v in vs], axis=1))


def _to_passes(aT):
    F = aT.shape[0]
    return np.ascontiguousarray(aT.reshape(F // 128, 128, NPASS, TP).transpose(2, 1, 0, 3))


def _from_passes(a):
    kc = a.shape[2]
    return a.transpose(2, 1, 0, 3).reshape(kc * 128, NPASS * TP)


def kernel(x, c, w_ada, b_ada, g_pre_mix, g_post_mix, w_in, sinks, w_br_a, w_br_b, w_br_c,
           w_out, g_pre_ffn, g_post_ffn, w_gate, w_up, w_down):
    P = dict(g_pre_mix=np.asarray(g_pre_mix), g_post_mix=np.asarray(g_post_mix), g_pre_ffn=np.asarray(g_pre_ffn),
             g_post_ffn=np.asarray(g_post_ffn))
    x = np.asarray(x); c = np.asarray(c); w_ada = np.asarray(w_ada); b_ada = np.asarray(b_ada)
    w_in = np.asarray(w_in); sinks = np.asarray(sinks)
    mod = _run_mod(c, w_ada, b_ada)
    xT = np.ascontiguousarray(x[0].T)
    hs = [_to_passes(xT[:, 2048 * cc:2048 * cc + 2048]) for cc in range(8)]
    nc0 = _get("p0", lambda: build_dense(False, True))
    v0 = _make_vecs(P, mod, 0, first=True)
    res = _run(nc0, [dict(hT=hs[cc], vecs=v0) for cc in range(8)])
    us = [res[cc]["uT_out"] for cc in range(8)]
    consts = [att_consts(cc) for cc in range(8)]
    for l in range(4):
        uT = np.ascontiguousarray(np.concatenate([_from_passes(np.asarray(u)) for u in us], axis=1))
        nca = _get("att", build_att)
        in_maps = []
        for cc in range(8):
            m = dict(uT=uT)
            m.update(consts[cc])
            m.update(att_weights(w_in[l], sinks[l], cc))
            in_maps.append(m)
        ra = _run(nca, in_maps)
        oT = np.concatenate([np.asarray(ra[cc]["oa"]) for cc in range(8)] + [np.asarray(ra[cc]["ob"]) for cc in range(8)]
                            + [np.asarray(ra[cc]["oc"]) for cc in range(4)], axis=0)
        last = (l == 3)
        ncd = _get("dense_last" if last else "dense", lambda: build_dense(True, not last))
        wts = dict(w_g=blockW(w_in[l][:, 4608:7680]),
                   w_br=blockW(np.concatenate([np.asarray(w_br_a[l]), np.asarray(w_br_b[l]), np.asarray(w_br_c[l])], axis=0)),
                   w_out=blockW(np.asarray(w_out[l])), w_gate=blockW(np.asarray(w_gate[l])), w_up=blockW(np.asarray(w_up[l])),
                   w_down=blockW(np.asarray(w_down[l])))
        vl = _make_vecs(P, mod, l)
        in_maps = []
        for cc in range(8):
            m = dict(hT=hs[cc], oT=_to_passes(np.ascontiguousarray(oT[:, 2048 * cc:2048 * cc + 2048])), vecs=vl)
            m.update(wts)
            in_maps.append(m)
        rd = _run(ncd, in_maps)
        hs = [np.asarray(rd[cc]["hT_out"]) for cc in range(8)]
        if not last:
            us = [rd[cc]["uT_out"] for cc in range(8)]
    hT = np.concatenate([_from_passes(h) for h in hs], axis=1)
    return np.ascontiguousarray(hT.T)[None].astype(np.float32)
```
